# Optimizing a Trainium2 kernel written in Bass

```python
import math
import jax, jax.numpy as jnp
from jax import lax
import numpy as np

D_MODEL = 1024
BATCH = 4
SEQ = 4096
DEPTH = 2

GRID_W = 64
CTX_LEN = 256
N_MIXERS = 2
S5_H = 16
S5_G = D_MODEL // S5_H
S5_P = 64
DT_MIN, DT_MAX = 1e-3, 1e-1
POOL_WINDOWS = (2, 4, 8, 16)
POOL_G = len(POOL_WINDOWS)
POOL_DG = D_MODEL // POOL_G
N_EXPERTS = 16
N_EXPERT_GROUPS = 4
EXPERTS_PER_GROUP = N_EXPERTS // N_EXPERT_GROUPS
TOP_K = 2
D_FF = 1024
N_S5_LAYERS = (DEPTH + 1) // 2
N_POOL_LAYERS = DEPTH // 2
DN_ALPHA = (2 * DEPTH) ** 0.25
DN_BETA = (8 * DEPTH) ** -0.25
LN_EPS = 1e-5

kernel_name = 'hybrid_s5_pool_moe_dit'


def _layer_norm(x, g, b):
    xf = x.astype(jnp.float32)
    mu = jnp.mean(xf, axis=-1, keepdims=True)
    var = jnp.mean(jnp.square(xf - mu), axis=-1, keepdims=True)
    y = (xf - mu) * lax.rsqrt(var + LN_EPS)
    return (y * g.astype(jnp.float32) + b.astype(jnp.float32)).astype(x.dtype)


def _modulation(cond, w, b):
    m = jnp.dot(jax.nn.silu(cond), w) + b
    return jnp.split(m, 6, axis=-1)


def _s5_discretize(lam_re, lam_im, log_dt, b_re, b_im):
    f32 = jnp.float32
    lr = lam_re.astype(f32)
    li = lam_im.astype(f32)
    dt = jnp.exp(log_dt.astype(f32))[:, None]
    mag = jnp.exp(lr * dt)
    ar = mag * jnp.cos(li * dt)
    ai = mag * jnp.sin(li * dt)
    den = lr * lr + li * li
    nr = ar - 1.0
    fr = (nr * lr + ai * li) / den
    fi = (ai * lr - nr * li) / den
    br_, bi_ = b_re.astype(f32), b_im.astype(f32)
    bbr = fr[..., None] * br_ - fi[..., None] * bi_
    bbi = fr[..., None] * bi_ + fi[..., None] * br_
    return ar, ai, bbr, bbi


def _complex_scan_combine(e1, e2):
    a1r, a1i, b1r, b1i = e1
    a2r, a2i, b2r, b2i = e2
    return (a2r * a1r - a2i * a1i,
            a2r * a1i + a2i * a1r,
            a2r * b1r - a2i * b1i + b2r,
            a2r * b1i + a2i * b1r + b2i)


def _s5_scan(u, ar, ai, bbr, bbi, h0, reverse):
    n = u.shape[1]
    bur = jnp.einsum('blgh,gph->blgp', u, bbr)
    bui = jnp.einsum('blgh,gph->blgp', u, bbi)
    a_shape = (1, n) + ar.shape
    elems = (jnp.broadcast_to(ar, a_shape), jnp.broadcast_to(ai, a_shape), bur, bui)
    acr, aci, xr, xi = lax.associative_scan(_complex_scan_combine, elems, reverse=reverse, axis=1)
    if h0 is not None:
        h0r = h0[0][:, None]
        h0i = h0[1][:, None]
        xr = xr + acr * h0r - aci * h0i
        xi = xi + acr * h0i + aci * h0r
    return xr, xi


def _s5_readout(xr, xi, c_re, c_im):
    return jnp.einsum('blgp,ghp->blgh', xr, c_re) - jnp.einsum('blgp,ghp->blgh', xi, c_im)


def _glu(y, w_val, w_gate, dtype):
    y = jax.nn.gelu(y).astype(dtype)
    return jnp.dot(y, w_val) * jax.nn.sigmoid(jnp.dot(y, w_gate))


def _s5_mixer(h_lat, h_ctx, lam_re, lam_im, log_dt, b_re, b_im, c_re, c_im, d_skip, w_val, w_gate, ctx_out):
    f32 = jnp.float32
    bsz, n_lat, _ = h_lat.shape
    n_ctx = h_ctx.shape[1]
    u_lat = h_lat.astype(f32).reshape(bsz, n_lat, S5_G, S5_H)
    u_ctx = h_ctx.astype(f32).reshape(h_ctx.shape[0], n_ctx, S5_G, S5_H)
    d = d_skip.astype(f32).reshape(S5_G, S5_H)
    y_lat = d * u_lat
    y_ctx = d * u_ctx if ctx_out else None
    for direction, reverse in ((0, False), (1, True)):
        ar, ai, bbr, bbi = _s5_discretize(lam_re[direction], lam_im[direction], log_dt[direction],
                                          b_re[direction], b_im[direction])
        cr = c_re[direction].astype(f32)
        ci = c_im[direction].astype(f32)
        xr_c, xi_c = _s5_scan(u_ctx, ar, ai, bbr, bbi, None, reverse)
        end = 0 if reverse else n_ctx - 1
        xr, xi = _s5_scan(u_lat, ar, ai, bbr, bbi, (xr_c[:, end], xi_c[:, end]), reverse)
        y_lat = y_lat + _s5_readout(xr, xi, cr, ci)
        if ctx_out:
            y_ctx = y_ctx + _s5_readout(xr_c, xi_c, cr, ci)
    out_lat = _glu(y_lat.reshape(bsz, n_lat, D_MODEL), w_val, w_gate, h_lat.dtype)
    out_ctx = _glu(y_ctx.reshape(bsz, n_ctx, D_MODEL), w_val, w_gate, h_ctx.dtype) if ctx_out else None
    return out_lat, out_ctx


def _box_sum(x, k, axis):
    n = x.shape[axis]
    cs = jnp.cumsum(x, axis=axis)
    cs = jnp.concatenate([jnp.zeros_like(lax.slice_in_dim(cs, 0, 1, axis=axis)), cs], axis=axis)
    t = jnp.arange(n)
    lo = jnp.clip(t - k // 2, 0, n)
    hi = jnp.clip(t + k - k // 2, 0, n)
    s = jnp.take(cs, hi, axis=axis) - jnp.take(cs, lo, axis=axis)
    shape = [1] * x.ndim
    shape[axis] = n
    return s, (hi - lo).astype(jnp.float32).reshape(shape)


def _pool_mixer(h, w_grp, scale, rows):
    bsz, n, d = h.shape
    hf = h.astype(jnp.float32)
    view = hf.reshape(bsz, rows, GRID_W, d) if rows is not None else hf
    parts = []
    for g, k in enumerate(POOL_WINDOWS):
        xg = view[..., g * POOL_DG:(g + 1) * POOL_DG]
        if rows is not None:
            s, cnt_c = _box_sum(xg, k, 2)
            s, cnt_r = _box_sum(s, k, 1)
            mean = s / (cnt_c * cnt_r)
        else:
            s, cnt = _box_sum(xg, k, 1)
            mean = s / cnt
        parts.append((mean - xg).reshape(bsz, n, POOL_DG))
    pooled = jnp.stack(parts, axis=2).astype(h.dtype)
    out = jnp.einsum('bngc,gcd->bngd', pooled, w_grp).reshape(bsz, n, d)
    return out * scale


def _moe(h, router_w, router_b, w_gate, w_up, w_down):
    f32 = jnp.float32
    shp = h.shape
    t = h.reshape(-1, shp[-1])
    logits = jnp.dot(t, router_w).astype(f32) + router_b.astype(f32)
    scores = jax.nn.softmax(logits, axis=-1)
    sg = scores.reshape(-1, N_EXPERT_GROUPS, EXPERTS_PER_GROUP)
    group_score = jnp.sum(lax.top_k(sg, TOP_K)[0], axis=-1)
    best = jnp.argmax(group_score, axis=-1)
    in_group = jnp.take_along_axis(sg, best[:, None, None], axis=1)[:, 0]
    top_w, top_i = lax.top_k(in_group, TOP_K)
    top_w = top_w / jnp.sum(top_w, axis=-1, keepdims=True)
    expert_idx = best[:, None] * EXPERTS_PER_GROUP + top_i
    combine = jnp.sum(jax.nn.one_hot(expert_idx, N_EXPERTS, dtype=f32) * top_w[..., None], axis=1)
    out = jnp.zeros(t.shape, f32)
    for e in range(N_EXPERTS):
        a = jax.nn.silu(jnp.dot(t, w_gate[e])) * jnp.dot(t, w_up[e])
        out = out + combine[:, e:e + 1] * jnp.dot(a, w_down[e]).astype(f32)
    return out.astype(h.dtype).reshape(shp)


def setup_inputs(seed: int = 0) -> dict:
    key = jax.random.key(seed)
    ks = jax.random.split(key, 26)
    f32 = jnp.float32

    def nrm(k, shape, s):
        return jax.random.normal(k, shape, f32) * s

    D, G, P, H, E, F = D_MODEL, S5_G, S5_P, S5_H, N_EXPERTS, D_FF
    n_idx = jnp.arange(P, dtype=f32)
    return {
        'x': nrm(ks[0], (BATCH, SEQ, D), 1.0),
        'c': nrm(ks[1], (BATCH, D), 1.0),
        'ctx': nrm(ks[2], (BATCH, CTX_LEN, D), 1.0),
        'c_ctx': nrm(ks[3], (D,), 1.0),
        'mod_w': nrm(ks[4], (DEPTH, D, 6 * D), 0.5 * D ** -0.5),
        'mod_b': nrm(ks[5], (DEPTH, 6 * D), 0.02),
        'ln_g': 1.0 + nrm(ks[6], (DEPTH, 2, D), 0.02),
        'ln_b': nrm(ks[7], (DEPTH, 2, D), 0.02),
        's5_lam_re': -0.5 + nrm(ks[8], (N_S5_LAYERS, 2, G, P), 0.01),
        's5_lam_im': math.pi * n_idx + nrm(ks[9], (N_S5_LAYERS, 2, G, P), 0.01),
        's5_log_dt': jax.random.uniform(ks[10], (N_S5_LAYERS, 2, G), f32, math.log(DT_MIN), math.log(DT_MAX)),
        's5_b_re': nrm(ks[11], (N_S5_LAYERS, 2, G, P, H), (2 * H) ** -0.5),
        's5_b_im': nrm(ks[12], (N_S5_LAYERS, 2, G, P, H), (2 * H) ** -0.5),
        's5_c_re': nrm(ks[13], (N_S5_LAYERS, 2, G, H, P), P ** -0.5),
        's5_c_im': nrm(ks[14], (N_S5_LAYERS, 2, G, H, P), P ** -0.5),
        's5_d': nrm(ks[15], (N_S5_LAYERS, D), 1.0),
        's5_w_val': nrm(ks[16], (N_S5_LAYERS, D, D), DN_BETA * D ** -0.5),
        's5_w_gate': nrm(ks[17], (N_S5_LAYERS, D, D), D ** -0.5),
        'pool_w': nrm(ks[18], (N_POOL_LAYERS, POOL_G, POOL_DG, POOL_DG), DN_BETA * POOL_DG ** -0.5),
        'pool_scale': 1.0 + nrm(ks[19], (N_POOL_LAYERS, D), 0.02),
        'router_w': nrm(ks[20], (D, E), D ** -0.5),
        'router_b': nrm(ks[21], (E,), 0.01),
        'moe_w_gate': nrm(ks[22], (DEPTH, E, D, F), D ** -0.5),
        'moe_w_up': nrm(ks[23], (DEPTH, E, D, F), D ** -0.5),
        'moe_w_down': nrm(ks[24], (DEPTH, E, F, D), DN_BETA * F ** -0.5),
    }


def reference(x, c, ctx, c_ctx, mod_w, mod_b, ln_g, ln_b, s5_lam_re, s5_lam_im, s5_log_dt, s5_b_re, s5_b_im,
              s5_c_re, s5_c_im, s5_d, s5_w_val, s5_w_gate, pool_w, pool_scale, router_w, router_b,
              moe_w_gate, moe_w_up, moe_w_down):
    rows = x.shape[1] // GRID_W
    cond_lat = c[:, None, :]
    cond_ctx = c_ctx[None, None, :]
    is_s5 = [i % N_MIXERS == 0 for i in range(DEPTH)]
    for i in range(DEPTH):
        j = i // N_MIXERS
        ctx_active = any(is_s5[i:])
        ctx_advance = any(is_s5[i + 1:])
        sh1, sc1, g1, sh2, sc2, g2 = _modulation(cond_lat, mod_w[i], mod_b[i])
        h = x * (1.0 + sc1) + sh1
        if ctx_active:
            csh1, csc1, cg1, csh2, csc2, cg2 = _modulation(cond_ctx, mod_w[i], mod_b[i])
            hc = ctx * (1.0 + csc1) + csh1
        if is_s5[i]:
            m, mc = _s5_mixer(h, hc, s5_lam_re[j], s5_lam_im[j], s5_log_dt[j], s5_b_re[j], s5_b_im[j],
                              s5_c_re[j], s5_c_im[j], s5_d[j], s5_w_val[j], s5_w_gate[j], ctx_advance)
        else:
            m = _pool_mixer(h, pool_w[j], pool_scale[j], rows)
            mc = _pool_mixer(hc, pool_w[j], pool_scale[j], None) if ctx_advance else None
        x = _layer_norm(DN_ALPHA * x + g1 * m, ln_g[i, 0], ln_b[i, 0])
        h = x * (1.0 + sc2) + sh2
        x = _layer_norm(DN_ALPHA * x + g2 * _moe(h, router_w, router_b, moe_w_gate[i], moe_w_up[i], moe_w_down[i]),
                        ln_g[i, 1], ln_b[i, 1])
        if ctx_advance:
            ctx = _layer_norm(DN_ALPHA * ctx + cg1 * mc, ln_g[i, 0], ln_b[i, 0])
            hc = ctx * (1.0 + csc2) + csh2
            ctx = _layer_norm(DN_ALPHA * ctx + cg2 * _moe(hc, router_w, router_b, moe_w_gate[i], moe_w_up[i],
                                                          moe_w_down[i]), ln_g[i, 1], ln_b[i, 1])
    return x
```

```python
from contextlib import ExitStack
import math
import numpy as np
import concourse.bass as bass
import concourse.mybir as mybir
from concourse.bass_utils import run_bass_kernel_spmd

F32 = mybir.dt.float32
BF16 = mybir.dt.bfloat16
I32 = mybir.dt.int32
ALU = mybir.AluOpType
AF = mybir.ActivationFunctionType

D = 1024
NCTX = 256
NOWN = 2048
NHALO = 512
NT0 = NOWN + NHALO
SEQLEN = NCTX + 2 * NOWN + NCTX
OFF_HALO = NCTX + NOWN - NHALO
OFF_OWN = NCTX + NOWN
OFF_CTX1 = NCTX + 2 * NOWN
G, P_, H = 64, 64, 16
E = 16
ALPHA = 4.0 ** 0.25
EPS = 1e-5
POOLK = (2, 4, 8, 16)
TWO_PI = 2.0 * math.pi

DEBUG = False
STAGE = 99
import os
GLUSUB = int(os.environ.get("GLUSUB", "9"))

ENGS = ("pe", "act", "dve", "pool")
NDMA = 4
EPOCH = 24000


class Sched:
    def __init__(self, nc, stack):
        self.nc = nc
        self.stack = stack
        self.eng = {"pe": nc.tensor, "act": nc.scalar, "dve": nc.vector, "pool": nc.gpsimd, "sp": nc.sync}
        self.sem = {}
        self.cnt = {}
        self.epoch = {}
        for e in ENGS:
            self.epoch[e] = 0
            self.sem[(e, 0)] = stack.enter_context(nc.semaphore("s_%s0" % e))
            self.cnt[e] = 0
        self.dsem, self.dcnt, self.dring = {}, {}, {}
        for q in ("sp", "act", "pool"):
            self.dsem[q] = [stack.enter_context(nc.semaphore("d_%s%d" % (q, i))) for i in range(NDMA)]
            self.dcnt[q] = [0] * NDMA
            self.dring[q] = 0
        self.seen = {e: {} for e in self.eng}
        self.lastw = {}
        self.readers = {}
        self.nwaits = 0
        self.nops = 0

    def _semobj(self, key):
        return self.sem[(key[1], key[2])] if key[0] == "e" else self.dsem[key[1]][key[2]]

    def _wait(self, eng, tok):
        if tok is None:
            return
        key, val = tok
        if eng == "pe" and key[0] == "e" and key[1] == "pe":
            return
        if self.seen[eng].get(key, 0) >= val:
            return
        self.eng[eng].wait_ge(self._semobj(key), val)
        self.seen[eng][key] = val
        self.nwaits += 1

    def _deps(self, eng, reads, writes):
        for k in reads:
            self._wait(eng, self.lastw.get(k))
        for k in writes:
            self._wait(eng, self.lastw.get(k))
            for t in self.readers.get(k, {}).values():
                self._wait(eng, t)

    def _commit(self, tok, reads, writes):
        for k in reads:
            self.readers.setdefault(k, {})[tok[0]] = tok
        for k in writes:
            self.lastw[k] = tok
            self.readers[k] = {}

    def op(self, eng, fn, reads=(), writes=()):
        self._deps(eng, reads, writes)
        if self.cnt[eng] >= EPOCH:
            self.epoch[eng] += 1
            self.cnt[eng] = 0
            self.sem[(eng, self.epoch[eng])] = self.stack.enter_context(
                self.nc.semaphore("s_%s%d" % (eng, self.epoch[eng])))
        ins = fn()
        self.cnt[eng] += 1
        ep = self.epoch[eng]
        ins.then_inc(self.sem[(eng, ep)], 1)
        tok = (("e", eng, ep), self.cnt[eng])
        self._commit(tok, reads, writes)
        self.nops += 1
        return tok

    def dma(self, q, out, in_, reads=(), writes=(), **kw):
        self._deps(q, reads, writes)
        i = self.dring[q]
        self.dring[q] = (i + 1) % NDMA
        key = ("d", q, i)
        if self.dcnt[q][i] > 0:
            self._wait(q, (key, self.dcnt[q][i]))
        ins = self.eng[q].dma_start(out=out, in_=in_, **kw)
        self.dcnt[q][i] += 16
        ins.then_inc(self.dsem[q][i], 16)
        tok = (key, self.dcnt[q][i])
        self._commit(tok, reads, writes)
        return tok

    def barrier(self):
        for e in ("pe", "act", "dve", "pool", "sp"):
            self.wait_all(e)

    def wait_all(self, eng):
        for e in ENGS:
            if self.cnt[e] > 0:
                self._wait(eng, (("e", e, self.epoch[e]), self.cnt[e]))
        for q in self.dsem:
            for i in range(NDMA):
                if self.dcnt[q][i] > 0:
                    self._wait(eng, (("d", q, i), self.dcnt[q][i]))


def rev_last(ap):
    pat = [list(x) for x in ap.ap]
    step, cnt = pat[-1]
    off = ap.offset + step * (cnt - 1)
    pat[-1] = [-step, cnt]
    return bass.AP(ap.tensor, off, pat)


class Ctx:
    pass


def build_program():
    nc = bass.Bass("TRN2", target_bir_lowering=False)
    K = Ctx()
    K.nc = nc

    def din(name, shape, dt=F32):
        return nc.dram_tensor(name, list(shape), dt, kind="ExternalInput").ap()

    def dscr(name, shape, dt=F32, dbg=False):
        kind = "ExternalOutput"
        return nc.dram_tensor(name, list(shape), dt, kind=kind).ap()

    I = Ctx()
    I.seq = din("seq", [SEQLEN, D])
    I.cvec = din("cvec", [2, D])
    I.mod_w = din("mod_w", [2, D, 6 * D])
    I.mod_b = din("mod_b", [2, 6 * D])
    I.ln_g = din("ln_g", [2, 2, D])
    I.ln_b = din("ln_b", [2, 2, D])
    I.lam_re = din("lam_re", [2, G, P_])
    I.lam_im = din("lam_im", [2, G, P_])
    I.log_dt = din("log_dt", [2, G])
    I.b_re = din("b_re", [2, G, P_, H])
    I.b_im = din("b_im", [2, G, P_, H])
    I.c_re = din("c_re", [2, G, H, P_])
    I.c_im = din("c_im", [2, G, H, P_])
    I.s5_d = din("s5_d", [D])
    I.w_val = din("w_val", [D, D])
    I.w_gate = din("w_gate", [D, D])
    I.pool_w = din("pool_w", [4, 256, 256])
    I.pool_scale = din("pool_scale", [D])
    I.router_w = din("router_w", [D, E])
    I.router_b = din("router_b", [E])
    if STAGE >= 3:
        I.moe_wg = din("moe_wg", [2, E, D, D])
        I.moe_wu = din("moe_wu", [2, E, D, D])
        I.moe_wd = din("moe_wd", [2, E, D, D])
    I.ident = din("ident", [128, 128])
    I.maskp = din("maskp", [128, 128])
    I.maskq = din("maskq", [128, 128])
    I.invc = din("invc", [4, 64])
    I.invr = din("invr", [4, 32])
    I.sel = din("sel", [2])
    out = nc.dram_tensor("out", [NOWN, D], F32, kind="ExternalOutput").ap()

    Sc = Ctx()
    Sc.MOD = dscr("MOD", [2, 2, 6 * D], dbg=True)
    Sc.Y1 = dscr("Y1", [NT0, D])
    Sc.YG = dscr("YG", [NT0, D], BF16, dbg=True)
    Sc.X1 = [dscr("X1_0", [NT0, D], dbg=True), dscr("X1_1", [NOWN, D], dbg=True)]
    Sc.H2T = [dscr("H2T_0", [8, 128, NT0], BF16), dscr("H2T_1", [8, 128, NOWN], BF16)]
    Sc.CMB = [dscr("CMB_0", [NT0, E], dbg=True), dscr("CMB_1", [NOWN, E], dbg=True)]
    Sc.X2 = dscr("X2", [NT0, D], dbg=True)

    with ExitStack() as top:
        S = Sched(nc, top)
        K.S = S
        K.ps = [top.enter_context(nc.psum_tensor("ps%d" % i, [128, 512], F32)) for i in range(7)]
        K.psb = top.enter_context(nc.psum_tensor("psb", [128, 1024], BF16))
        K.psf = K.psb[:, :].bitcast(F32)
        K.ident = top.enter_context(nc.sbuf_tensor("identS", [128, 128], F32))
        K.identb = top.enter_context(nc.sbuf_tensor("identbS", [128, 128], BF16))
        S.dma("sp", K.ident[:], I.ident[:, :], writes=["ident"])
        S.op("dve", lambda: nc.vector.tensor_copy(K.identb[:], K.ident[:]), reads=["ident"], writes=["identb"])

        if STAGE >= 0:
            phase_mod(K, I, Sc)
        if STAGE >= 1:
            phase_s5(K, I, Sc)
        if STAGE >= 2:
            phase_glu(K, I, Sc)
        if STAGE >= 3:
            phase_moe(K, I, Sc, 0, NT0, Sc.X2, 0)
        if STAGE >= 4:
            phase_pool(K, I, Sc)
        if STAGE >= 5:
            phase_moe(K, I, Sc, 1, NOWN, out, 0)
        else:
            with ExitStack() as st:
                z = st.enter_context(nc.sbuf_tensor("zz", [128, D], F32))
                S.op("dve", lambda: nc.vector.memset(z[:], 0.0), writes=["zz"])
                for t in range(NOWN // 128):
                    S.dma("sp", out[t * 128:(t + 1) * 128, :], z[:], reads=["zz"])
        S.wait_all("sp")
        K.stats = (S.nops, S.nwaits)
    return nc


def bcast_load(K, st, name, src_row):
    nc, S = K.nc, K.S
    n = src_row.shape[-1]
    t = st.enter_context(nc.sbuf_tensor(name, [128, n], F32))
    S.dma("sp", t[:], src_row.partition_broadcast(128), writes=[name])
    return t


def dbg_dump(K, name, ap, reads):
    if not DEBUG:
        return
    nc, S = K.nc, K.S
    t = nc.dram_tensor("DBG_" + name, list(ap.shape), ap.dtype, kind="ExternalOutput").ap()
    S.dma("sp", t, ap, reads=list(reads))


def bload(K, st, name, src_row, reads=()):
    nc, S = K.nc, K.S
    n = src_row.shape[-1]
    t = st.enter_context(nc.sbuf_tensor(name, [128, n], F32))
    S.dma("sp", t[:], src_row.partition_broadcast(128), reads=list(reads), writes=[name])
    return t


def phase_mod(K, I, Sc):
    nc, S = K.nc, K.S
    with ExitStack() as st:
        cT = st.enter_context(nc.sbuf_tensor("cT", [128, 8, 2], F32))
        sT = st.enter_context(nc.sbuf_tensor("sT", [128, 8, 2], F32))
        for r in range(2):
            S.dma("sp", cT[:, :, r], I.cvec[r, :].rearrange("(k p) -> p k", p=128), writes=["cT"],
                  allow_slow_non_contiguous=True)
        S.op("act", lambda: nc.scalar.activation(sT[:], cT[:], AF.Silu), reads=["cT"], writes=["sT"])
        wb = [st.enter_context(nc.sbuf_tensor("modw%d" % i, [128, 3072], F32)) for i in range(2)]
        mb = st.enter_context(nc.sbuf_tensor("modb", [2, 3072], F32))
        mrow = st.enter_context(nc.sbuf_tensor("mrow", [2, 3072], F32))
        it = 0
        for l in range(2):
            for cg in range(2):
                S.dma("sp", mb[:], I.mod_b[l, cg * 3072:(cg + 1) * 3072].partition_broadcast(2), writes=["modb"])
                for kc in range(8):
                    w = wb[it % 2]
                    wk = "modw%d" % (it % 2)
                    it += 1
                    S.dma("sp" if kc % 2 == 0 else "act", w[:], I.mod_w[l, kc * 128:(kc + 1) * 128, cg * 3072:(cg + 1) * 3072],
                          writes=[wk])
                    for cb in range(6):
                        S.op("pe", lambda: nc.tensor.matmul(K.ps[cb][0:2, :], sT[:, kc, :], w[:, cb * 512:(cb + 1) * 512],
                                                            start=(kc == 0), stop=(kc == 7)),
                             reads=[wk, "sT"], writes=["ps%d" % cb])
                for cb in range(6):
                    S.op("dve", lambda: nc.vector.tensor_tensor(mrow[:, cb * 512:(cb + 1) * 512], K.ps[cb][0:2, :],
                                                                mb[:, cb * 512:(cb + 1) * 512], ALU.add),
                         reads=["ps%d" % cb, "modb"], writes=["mrow"])
                S.dma("sp", Sc.MOD[l, :, cg * 3072:(cg + 1) * 3072], mrow[:], reads=["mrow"], writes=["MOD"])
        S.barrier()


def _box_counts(n, k, lo_shift):
    t = np.arange(n)
    lo = np.clip(t - k // 2, 0, n)
    hi = np.clip(t + k - k // 2, 0, n)
    return (hi - lo).astype(np.float32)


def make_in_maps(inp):
    f32 = np.float32
    x = np.asarray(inp["x"], f32)
    c = np.asarray(inp["c"], f32)
    ctx = np.asarray(inp["ctx"], f32)
    c_ctx = np.asarray(inp["c_ctx"], f32)
    ident = np.eye(128, dtype=f32)
    s_idx = np.arange(128) // 16
    maskp = (s_idx[:, None] <= s_idx[None, :]).astype(f32)
    maskq = (s_idx[:, None] >= s_idx[None, :]).astype(f32)
    shared = {
        "mod_w": np.ascontiguousarray(inp["mod_w"], f32), "mod_b": np.ascontiguousarray(inp["mod_b"], f32),
        "ln_g": np.ascontiguousarray(inp["ln_g"], f32), "ln_b": np.ascontiguousarray(inp["ln_b"], f32),
        "s5_d": np.ascontiguousarray(inp["s5_d"][0], f32),
        "w_val": np.ascontiguousarray(inp["s5_w_val"][0], f32), "w_gate": np.ascontiguousarray(inp["s5_w_gate"][0], f32),
        "pool_w": np.ascontiguousarray(inp["pool_w"][0], f32), "pool_scale": np.ascontiguousarray(inp["pool_scale"][0], f32),
        "router_w": np.ascontiguousarray(inp["router_w"], f32), "router_b": np.ascontiguousarray(inp["router_b"], f32),
        "moe_wg": np.ascontiguousarray(inp["moe_w_gate"], f32), "moe_wu": np.ascontiguousarray(inp["moe_w_up"], f32),
        "moe_wd": np.ascontiguousarray(inp["moe_w_down"], f32),
        "ident": ident, "maskp": maskp, "maskq": maskq,
    }
    maps = []
    for k in range(8):
        b, half = k // 2, k % 2
        if half == 1:
            own, oth, cf = x[b, NOWN:], x[b, :NOWN], ctx[b]
            dirs = [0, 1]
        else:
            own, oth, cf = x[b, :NOWN][::-1], x[b, NOWN:][::-1], ctx[b][::-1]
            dirs = [1, 0]
        m = dict(shared)
        m["seq"] = np.ascontiguousarray(np.concatenate([cf, oth, own, cf], axis=0))
        m["cvec"] = np.ascontiguousarray(np.stack([c[b], c_ctx]))
        for nm, src in (("lam_re", "s5_lam_re"), ("lam_im", "s5_lam_im"), ("log_dt", "s5_log_dt"), ("b_re", "s5_b_re"),
                        ("b_im", "s5_b_im"), ("c_re", "s5_c_re"), ("c_im", "s5_c_im")):
            m[nm] = np.ascontiguousarray(np.asarray(inp[src], f32)[0][dirs])
        invc = np.zeros((4, 64), f32)
        invr = np.zeros((4, 32), f32)
        for gi, kk in enumerate(POOLK):
            cc = _box_counts(64, kk, 0)
            if half == 1:
                invc[gi] = 1.0 / cc
                invr[gi] = 1.0 / cc[32:64]
            else:
                invc[gi] = 1.0 / cc[::-1]
                invr[gi] = 1.0 / cc[0:32][::-1]
        m["invc"], m["invr"] = invc, invr
        m["sel"] = np.array([1.0, 0.0] if half == 1 else [0.0, 1.0], f32)
        maps.append(m)
    return maps


_CACHE = {}


def kernel(**inputs):
    key = (DEBUG, STAGE)
    if key not in _CACHE:
        _CACHE[key] = build_program()
    nc = _CACHE[key]
    maps = make_in_maps(inputs)
    names = set()
    for alloc in nc.allocations:
        try:
            if alloc.kind == "ExternalInput":
                names.add(alloc.memorylocations[0].name)
        except Exception:
            pass
    if names:
        maps = [{k: v for k, v in m.items() if k in names} for m in maps]
    res = run_bass_kernel_spmd(nc, maps, core_ids=list(range(8)))
    kernel.last = res
    outp = np.zeros((4, 2 * NOWN, D), np.float32)
    for k in range(8):
        b, half = k // 2, k % 2
        o = res.results[k]["out"]
        if half == 1:
            outp[b, NOWN:] = o
        else:
            outp[b, :NOWN] = o[::-1]
    return outp


def phase_s5(K, I, Sc):
    for dirn in (0, 1):
        with ExitStack() as st:
            O = s5_build_ops(K, I, Sc, st, dirn)
            s5_pass(K, I, Sc, st, dirn, O)
            K.S.barrier()


def s5_build_ops(K, I, Sc, st, dirn):
    nc, S = K.nc, K.S
    sfx = "_%d" % dirn
    O = Ctx()

    def sb(stack, name, shape, dt=F32):
        return stack.enter_context(nc.sbuf_tensor(name + sfx, list(shape), dt))

    O.WS = sb(st, "WS", [128, G, 2, 64], BF16)
    O.Mop = sb(st, "Mop", [128, G, 128], BF16)
    O.YS = sb(st, "YS", [128, 32, 2, 256], BF16)
    O.COS = sb(st, "COS", [128, 32, 65])
    O.SIN = sb(st, "SIN", [128, 32, 65])
    O.MULT = sb(st, "MULT", [128, 32, 65])
    O.tab = sb(st, "tab", [128, 5, G])
    kWS, kM, kYS, kROT, kTAB = "WS" + sfx, "Mop" + sfx, "YS" + sfx, "ROT" + sfx, "tab" + sfx

    def V(fn, r, w, eng="dve"):
        S.op(eng, fn, reads=r, writes=w)

    with ExitStack() as tt:
        rows = [Sc.MOD[0, 0, D:2 * D], Sc.MOD[0, 0, 0:D], Sc.MOD[0, 1, D:2 * D], Sc.MOD[0, 1, 0:D], I.s5_d[:]]
        for i, row in enumerate(rows):
            S.dma("sp", O.tab[0:16, i, :], row.rearrange("(g h) -> h g", h=16), reads=["MOD"], writes=[kTAB],
                  allow_slow_non_contiguous=True)
        for s_ in range(1, 8):
            S.dma("sp", O.tab[s_ * 16:(s_ + 1) * 16, :, :], O.tab[0:16, :, :], reads=[kTAB], writes=[kTAB + "r%d" % s_])
        allt = [kTAB] + [kTAB + "r%d" % s_ for s_ in range(1, 8)]
        for i in (0, 2):
            V(lambda: nc.vector.tensor_scalar_add(O.tab[:, i, :], O.tab[:, i, :], 1.0), allt, allt)

        LT = sb(tt, "LT", [64, 2, 128])
        lamA = sb(tt, "lamA", [128, 2, G])
        ldt = sb(tt, "ldt", [128, G])
        BA = sb(tt, "BA", [128, 2, G, H])
        CT = sb(tt, "CT", [128, 2, 8, 128])
        CA = sb(tt, "CA", [128, 2, G, H])
        for ri, arr in ((0, I.lam_re), (1, I.lam_im)):
            for dup in range(2):
                S.dma("sp", LT[:, ri, dup * 64:(dup + 1) * 64], arr[dirn, :, :], writes=["LT" + sfx])
        S.dma("sp", ldt[:], I.log_dt[dirn, :].partition_broadcast(128), writes=["ldt" + sfx])
        for ri, arr in ((0, I.b_re), (1, I.b_im)):
            for dup in range(2):
                S.dma("act", BA[dup * 64:(dup + 1) * 64, ri, :, :], arr[dirn].rearrange("g p h -> p g h"),
                      writes=["BA" + sfx])
        for ri, arr in ((0, I.c_re), (1, I.c_im)):
            for dup in range(2):
                S.dma("act", CT[:, ri, :, dup * 64:(dup + 1) * 64],
                      arr[dirn].rearrange("(gb gl) h p -> (gl h) gb p", gl=8), writes=["CT" + sfx])
        for ri in range(2):
            V(lambda: nc.tensor.transpose(K.ps[0][:, ri * 64:(ri + 1) * 64], LT[:, ri, :], K.ident[0:64, 0:64]),
              ["LT" + sfx, "ident"], ["ps0"], "pe")
        V(lambda: nc.vector.tensor_copy(lamA[:].rearrange("q r g -> q (r g)"), K.ps[0][:, 0:128]), ["ps0"], ["lamA" + sfx])
        for ri in range(2):
            for gq in range(2):
                for gb4 in range(4):
                    gb = gq * 4 + gb4
                    V(lambda: nc.tensor.transpose(K.ps[1 + gq][:, gb4 * 128:(gb4 + 1) * 128], CT[:, ri, gb, :], K.ident[:]),
                      ["CT" + sfx, "ident"], ["ps%d" % (1 + gq)], "pe")
                V(lambda: nc.vector.tensor_copy(CA[:, ri, gq * 32:(gq + 1) * 32, :].rearrange("q g h -> q (g h)"),
                                                K.ps[1 + gq][:, :]), ["ps%d" % (1 + gq)], ["CA" + sfx])

        sm = {}

        def T(name):
            if name not in sm:
                sm[name] = sb(tt, "sm_" + name, [128, G])
            return sm[name]

        kS = "small" + sfx

        def tt_(o, a, b, op):
            V(lambda: nc.vector.tensor_tensor(o, a, b, op), [kS, "lamA" + sfx, "ldt" + sfx], [kS])

        def ts_(o, a, s1, op):
            V(lambda: nc.vector.tensor_single_scalar(o, a, s1, op), [kS], [kS])

        lr, li = lamA[:, 0, :], lamA[:, 1, :]
        dtA = T("dt")
        V(lambda: nc.scalar.activation(dtA[:], ldt[:], AF.Exp), ["ldt" + sfx], [kS], "act")
        tt_(T("th")[:], li, dtA[:], ALU.mult)
        tt_(T("lrdt")[:], lr, dtA[:], ALU.mult)
        V(lambda: nc.scalar.activation(T("magP")[:], T("lrdt")[:], AF.Exp), [kS], [kS], "act")
        V(lambda: nc.scalar.activation(T("magN")[:], T("lrdt")[:], AF.Exp, scale=-1.0), [kS], [kS], "act")
        V(lambda: nc.scalar.activation(T("mag8")[:], T("lrdt")[:], AF.Exp, scale=8.0), [kS], [kS], "act")
        ts_(T("y")[:], T("th")[:], 1.0 / TWO_PI, ALU.mult)
        ni = tt.enter_context(nc.sbuf_tensor("ni" + sfx, [128, G], I32))
        for off, dst in ((0.0, "sin"), (0.25, "cos")):
            ts_(T("y2")[:], T("y")[:], off, ALU.add)
            V(lambda: nc.vector.tensor_copy(ni[:], T("y2")[:]), [kS], [kS])
            V(lambda: nc.vector.tensor_copy(T("nf")[:], ni[:]), [kS], [kS])
            tt_(T("r")[:], T("y2")[:], T("nf")[:], ALU.subtract)
            ts_(T("m")[:], T("r")[:], 0.5, ALU.is_gt)
            tt_(T("r")[:], T("r")[:], T("m")[:], ALU.subtract)
            ts_(T("m")[:], T("r")[:], -0.5, ALU.is_lt)
            tt_(T("r")[:], T("r")[:], T("m")[:], ALU.add)
            V(lambda: nc.scalar.activation(T(dst)[:], T("r")[:], AF.Sin, scale=6.28318), [kS], [kS], "act")

        PF = sb(tt, "PF", [128, 2, 9, G])
        PN = sb(tt, "PN", [128, 2, 8, G])

        def cmul(ore, oim, are, aim, bre, bim, t1, t2, r, w):
            V(lambda: nc.vector.tensor_tensor(t1, are, bre, ALU.mult), r, w)
            V(lambda: nc.vector.tensor_tensor(t2, aim, bim, ALU.mult), r, w)
            V(lambda: nc.vector.tensor_tensor(ore, t1, t2, ALU.subtract), r, w)
            V(lambda: nc.vector.tensor_tensor(t1, are, bim, ALU.mult), r, w)
            V(lambda: nc.vector.tensor_tensor(t2, aim, bre, ALU.mult), r, w)
            V(lambda: nc.vector.tensor_tensor(oim, t1, t2, ALU.add), r, w)

        for tab in (PF, PN):
            V(lambda: nc.vector.memset(tab[:, 0, 0, :], 1.0), [kS], [kS])
            V(lambda: nc.vector.memset(tab[:, 1, 0, :], 0.0), [kS], [kS])
        tt_(PF[:, 0, 1, :], T("magP")[:], T("cos")[:], ALU.mult)
        tt_(PF[:, 1, 1, :], T("magP")[:], T("sin")[:], ALU.mult)
        tt_(PN[:, 0, 1, :], T("magN")[:], T("cos")[:], ALU.mult)
        tt_(T("m")[:], T("magN")[:], T("sin")[:], ALU.mult)
        ts_(PN[:, 1, 1, :], T("m")[:], -1.0, ALU.mult)
        for k in range(2, 9):
            cmul(PF[:, 0, k, :], PF[:, 1, k, :], PF[:, 0, k - 1, :], PF[:, 1, k - 1, :], PF[:, 0, 1, :], PF[:, 1, 1, :],
                 T("t1")[:], T("t2")[:], [kS], [kS])
        for k in range(2, 8):
            cmul(PN[:, 0, k, :], PN[:, 1, k, :], PN[:, 0, k - 1, :], PN[:, 1, k - 1, :], PN[:, 0, 1, :], PN[:, 1, 1, :],
                 T("t1")[:], T("t2")[:], [kS], [kS])
        ts_(T("nr")[:], PF[:, 0, 1, :], -1.0, ALU.add)
        tt_(T("den")[:], lr, lr, ALU.mult)
        tt_(T("t1")[:], li, li, ALU.mult)
        tt_(T("den")[:], T("den")[:], T("t1")[:], ALU.add)
        V(lambda: nc.vector.reciprocal(T("rden")[:], T("den")[:]), [kS], [kS])
        tt_(T("t1")[:], T("nr")[:], lr, ALU.mult)
        tt_(T("t2")[:], PF[:, 1, 1, :], li, ALU.mult)
        tt_(T("t1")[:], T("t1")[:], T("t2")[:], ALU.add)
        tt_(T("fr")[:], T("t1")[:], T("rden")[:], ALU.mult)
        tt_(T("t1")[:], PF[:, 1, 1, :], lr, ALU.mult)
        tt_(T("t2")[:], T("nr")[:], li, ALU.mult)
        tt_(T("t1")[:], T("t1")[:], T("t2")[:], ALU.subtract)
        tt_(T("fi")[:], T("t1")[:], T("rden")[:], ALU.mult)
        Bb = sb(tt, "Bb", [128, 2, G, H])
        bt1 = sb(tt, "bt1", [128, G, H])
        bt2 = sb(tt, "bt2", [128, G, H])
        frb = T("fr")[:].unsqueeze(2).to_broadcast([128, G, H])
        fib = T("fi")[:].unsqueeze(2).to_broadcast([128, G, H])
        cmul(Bb[:, 0], Bb[:, 1], frb, fib, BA[:, 0], BA[:, 1], bt1[:], bt2[:], [kS, "BA" + sfx], [kS])

        r8 = sb(tt, "r8", [128, 32])
        e1 = sb(tt, "e1", [128, 2, 32])
        e2 = sb(tt, "e2", [128, 2, 32])
        rt1 = sb(tt, "rt1", [128, 32, 32])
        rt2 = sb(tt, "rt2", [128, 32, 32])
        kR = "rot" + sfx
        V(lambda: nc.vector.reciprocal(T("rm8")[:], T("mag8")[:]), [kS], [kS])
        tt_(T("u_re")[:], PF[:, 0, 8, :], T("rm8")[:], ALU.mult)
        tt_(T("u_im")[:], PF[:, 1, 8, :], T("rm8")[:], ALU.mult)
        for gpar in range(2):
            sl = slice(gpar * 64, (gpar + 1) * 64)

            def par(t):
                return t[sl, :].rearrange("q (a two) -> q a two", two=2)[:, :, gpar]
            V(lambda: nc.vector.tensor_copy(r8[sl, :], par(T("mag8"))), [kS], [kR])
            V(lambda: nc.vector.tensor_copy(e1[sl, 0, :], par(T("u_re"))), [kS], [kR])
            V(lambda: nc.vector.tensor_copy(e1[sl, 1, :], par(T("u_im"))), [kS], [kR])
        V(lambda: nc.vector.memset(O.COS[:, :, 0:1], 1.0), [kROT], [kROT])
        V(lambda: nc.vector.memset(O.SIN[:, :, 0:1], 0.0), [kROT], [kROT])
        cur, nxt = e1, e2
        m = 1
        while m <= 64:
            n = min(m, 65 - m)
            cb = lambda t, ri: t[:, ri, :].unsqueeze(2).to_broadcast([128, 32, n])
            cmul(O.COS[:, :, m:m + n], O.SIN[:, :, m:m + n], O.COS[:, :, 0:n], O.SIN[:, :, 0:n], cb(cur, 0), cb(cur, 1),
                 rt1[:, :, 0:n], rt2[:, :, 0:n], [kROT, kR], [kROT, kR])
            if m < 64:
                cmul(nxt[:, 0, :], nxt[:, 1, :], cur[:, 0, :], cur[:, 1, :], cur[:, 0, :], cur[:, 1, :],
                     rt1[:, 0, :], rt2[:, 0, :], [kR], [kR])
                cur, nxt = nxt, cur
            m *= 2
        V(lambda: nc.vector.memset(O.MULT[:, :, 0:1], 0.0), [kROT], [kROT])
        V(lambda: nc.vector.tensor_copy(O.MULT[:, :, 1:65], r8[:].unsqueeze(2).to_broadcast([128, 32, 64])), [kR, kROT], [kROT])

        NQ = 8
        big = [sb(tt, "big%d" % i, [128, NQ, 8, H]) for i in range(8)]
        Bsr, Bsi, Cjr, Cji, T1, T2, W1, W2 = big
        kB = ["big%d" % i + sfx for i in range(8)]
        tmpM = sb(tt, "tmpM", [128, 128])
        msk = sb(tt, "msk", [128, 128])
        S.dma("sp", msk[:], (I.maskp if dirn == 0 else I.maskq)[:, :], writes=["msk" + sfx])
        tabB, tabC = (PN, PF) if dirn == 0 else (PF, PN)
        SH = [128, NQ, 8, H]
        for qd in range(G // NQ):
            g0 = qd * NQ

            def powb(tab, ri, k0=0):
                return tab[:, ri, k0:k0 + 8, g0:g0 + NQ].rearrange("q s g -> q g s").unsqueeze(3).to_broadcast(SH)

            def gb_(tab3, ri):
                return tab3[:, ri, g0:g0 + NQ, :].unsqueeze(2).to_broadcast(SH)

            def gs_(t2d):
                return t2d[:, g0:g0 + NQ].unsqueeze(2).unsqueeze(3).to_broadcast(SH)
            rk = [kS, "CA" + sfx] + kB
            cmul(Bsr[:], Bsi[:], powb(tabB, 0), powb(tabB, 1), gb_(Bb, 0), gb_(Bb, 1), T1[:], T2[:], rk, kB[0:2] + kB[4:6])
            cmul(Cjr[:], Cji[:], powb(tabC, 0), powb(tabC, 1), gb_(CA, 0), gb_(CA, 1), T1[:], T2[:], rk, kB[2:4] + kB[4:6])
            V(lambda: nc.vector.tensor_single_scalar(W1[:], Bsi[:], -1.0, ALU.mult), [kB[1]], [kB[6]])
            for gl in range(NQ):
                g = g0 + gl
                bank = K.ps[3 + (gl // 4) % 2]
                bk = "ps%d" % (3 + (gl // 4) % 2)
                o = bank[:, (gl % 4) * 128:(gl % 4 + 1) * 128]
                f = lambda t: t[0:64, gl, :, :].rearrange("q s h -> q (s h)")
                V(lambda: nc.tensor.matmul(o, f(Bsr), f(Cjr), start=True, stop=False), [kB[0], kB[2]], [bk], "pe")
                V(lambda: nc.tensor.matmul(o, f(W1), f(Cji), start=False, stop=True), [kB[6], kB[3]], [bk], "pe")
                if dirn == 0:
                    V(lambda: nc.vector.tensor_tensor(tmpM[:], o, msk[:], ALU.mult), [bk, "msk" + sfx], ["tmpM" + sfx])
                    V(lambda: nc.vector.scalar_tensor_tensor(O.Mop[:, g, :], K.ident[:], O.tab[:, 4, g:g + 1], tmpM[:],
                                                             ALU.mult, ALU.add),
                      ["tmpM" + sfx, "ident"] + allt, [kM])
                else:
                    V(lambda: nc.vector.tensor_tensor(O.Mop[:, g, :], o, msk[:], ALU.mult), [bk, "msk" + sfx], [kM])
            if dirn == 0:
                cmul(W1[:], W2[:], gs_(PF[:, 0, 7, :]), gs_(PF[:, 1, 7, :]), Bsr[:], Bsi[:], T1[:], T2[:],
                     [kS] + kB, kB[4:8])
                wsa = (W1, W2)
                kW = (kB[6], kB[7])
            else:
                wsa = (Bsr, Bsi)
                kW = (kB[0], kB[1])
            for g4 in range(NQ // 4):
                bank, bk = K.ps[5], "ps5"
                for gq in range(4):
                    gl = g4 * 4 + gq
                    for ri in range(2):
                        V(lambda: nc.tensor.transpose(bank[:, (gq * 2 + ri) * 64:(gq * 2 + ri + 1) * 64],
                                                      wsa[ri][0:64, gl, :, :].rearrange("q s h -> q (s h)"),
                                                      K.ident[0:64, 0:64]),
                          [kW[ri], "ident"], [bk], "pe")
                gg = g0 + g4 * 4
                V(lambda: nc.scalar.copy(O.WS[:, gg:gg + 4, :, :].rearrange("q g r p -> q (g r p)"), bank[:, :]),
                  [bk], [kWS], "act")
            kk = 1 if dirn == 0 else 8
            cmul(W1[:], W2[:], gs_(PF[:, 0, kk, :]), gs_(PF[:, 1, kk, :]), Cjr[:], Cji[:], T1[:], T2[:],
                 [kS] + kB, kB[4:8])
            p0 = g0 // 2
            for gpar in range(2):
                sl = slice(gpar * 64, (gpar + 1) * 64)
                osl = slice((1 - gpar) * 64, (2 - gpar) * 64)
                src = lambda t: t[sl].rearrange("q (a two) s h -> q a two (s h)", two=2)[:, :, gpar, :]
                V(lambda: nc.vector.tensor_copy(O.YS[sl, p0:p0 + NQ // 2, 0, gpar * 128:(gpar + 1) * 128], src(W1)),
                  [kB[6]], [kYS])
                V(lambda: nc.vector.tensor_single_scalar(O.YS[sl, p0:p0 + NQ // 2, 1, gpar * 128:(gpar + 1) * 128], src(W2),
                                                         -1.0, ALU.mult), [kB[7]], [kYS])
                V(lambda: nc.gpsimd.memset(O.YS[sl, p0:p0 + NQ // 2, :, (1 - gpar) * 128:(2 - gpar) * 128], 0.0),
                  [], [kYS], "pool")
        if True:
            dbg_dump(K, "tab" + sfx, O.tab[:], allt)
            dbg_dump(K, "PF" + sfx, PF[:], [kS])
            dbg_dump(K, "PN" + sfx, PN[:], [kS])
            dbg_dump(K, "sincos" + sfx, T("sin")[:], [kS])
            dbg_dump(K, "Bb" + sfx, Bb[:], [kS])
            dbg_dump(K, "CA" + sfx, CA[:], ["CA" + sfx])
            dbg_dump(K, "COS" + sfx, O.COS[:], [kROT, kR])
            dbg_dump(K, "SIN" + sfx, O.SIN[:], [kROT, kR])
            dbg_dump(K, "MULT" + sfx, O.MULT[:], [kROT])
            dbg_dump(K, "Mop" + sfx, O.Mop[:], [kM])
            dbg_dump(K, "WS" + sfx, O.WS[:], [kWS])
            dbg_dump(K, "YS" + sfx, O.YS[:], [kYS])
        S.barrier()
    O.keys = (kWS, kM, kYS, kROT, allt)
    return O


def s5_pass(K, I, Sc, st, dirn, O):
    nc, S = K.nc, K.S
    sfx = "_%d" % dirn
    kWS, kM, kYS, kROT, allt = O.keys
    rev = (dirn == 1)

    def sb(name, shape, dt=F32):
        return st.enter_context(nc.sbuf_tensor(name + sfx, list(shape), dt))

    def V(fn, r, w, eng="dve"):
        S.op(eng, fn, reads=r, writes=w)

    xb = [sb("xb%d" % p, [64, 8, 128]) for p in range(2)]
    xr = [sb("xr%d" % p, [64, 8, 8, 16]) for p in range(2)]
    Rbs = [sb("Rb%d" % p, [128, G, 64], BF16) for p in range(2)]
    Z = sb("Z", [128, 2, 32, 65])
    ZS = sb("ZS", [128, 2, 32, 65])
    ft1 = sb("ft1", [128, 8, 64])
    ft2 = sb("ft2", [128, 8, 64])
    bt1 = sb("pbt1", [128, 32, 64])
    bt2 = sb("pbt2", [128, 32, 64])
    hist = sb("hist", [128, 2, 32, 64], BF16)
    ybuf = sb("ybuf", [64, 8, 128])
    y1b = sb("y1b", [64, 8, 128])
    ygb = sb("ygb", [64, 8, 128], BF16)
    V(lambda: nc.vector.memset(Z[:], 0.0), [], ["Z"])

    if dirn == 0:
        blocks = [(0, 32, True, False)] + [(NCTX + 512 * k, 64, False, k >= 3) for k in range(8)]
    else:
        blocks = [(OFF_CTX1, 32, True, False)] + [(NCTX + 512 * k, 64, False, True) for k in (7, 6, 5, 4, 3)]
    qcnt = [0]

    def front(bi):
        t0, nch, is_ctx, want = blocks[bi]
        isc, ish = (2, 3) if is_ctx else (0, 1)
        Rb, kRb = Rbs[bi % 2], "Rb%d" % (bi % 2)
        for hq in range(8):
            qp = qcnt[0] % 2
            qcnt[0] += 1
            xb_, xr_, kxb, kxr = xb[qp], xr[qp], "xb%d" % qp, "xr%d" % qp
            S.dma("sp", xb_[0:nch, :, :], I.seq[t0:t0 + nch * 8, hq * 128:(hq + 1) * 128].rearrange("(c s) d -> c s d", s=8),
                  writes=[kxb])
            V(lambda: nc.gpsimd.tensor_copy(xr_[0:nch].rearrange("c g s h -> c s g h"),
                                            xb_[0:nch].rearrange("c s (g h) -> c s g h", h=16)), [kxb], [kxr], "pool")
            for gq in range(1):
                bank, bk = (K.ps[0], "ps0") if hq % 2 == 0 else (K.psf, "psb")
                for gl in range(8):
                    g_loc = gl
                    V(lambda: nc.tensor.transpose(bank[:, gl * 64:gl * 64 + nch],
                                                  xr_[0:nch, g_loc, :, :].rearrange("c s h -> c (s h)"),
                                                  K.ident[0:nch, 0:nch]), [kxr, "ident"], [bk], "pe")
                for gl in range(8):
                    g = hq * 8 + gl
                    V(lambda: nc.scalar.activation(Rb[:, g, 0:nch], bank[:, gl * 64:gl * 64 + nch], AF.Identity,
                                                   bias=O.tab[:, ish, g:g + 1], scale=O.tab[:, isc, g:g + 1]),
                      [bk] + allt, [kRb], "act")

    def mid(bi):
        t0, nch, is_ctx, want = blocks[bi]
        Rb, kRb = Rbs[bi % 2], "Rb%d" % (bi % 2)
        sl0 = 65 - nch
        if is_ctx:
            V(lambda: nc.vector.memset(Z[:], 0.0), [], ["Z"])
        for sbk in range(4):
            pre, pim = K.ps[2 + 2 * (sbk % 2)], K.ps[3 + 2 * (sbk % 2)]
            kre, kim = "ps%d" % (2 + 2 * (sbk % 2)), "ps%d" % (3 + 2 * (sbk % 2))
            for pl in range(8):
                pair = sbk * 8 + pl
                for gpar in range(2):
                    g = 2 * pair + gpar
                    for ri, pst, kk in ((0, pre, kre), (1, pim, kim)):
                        V(lambda: nc.tensor.matmul(pst[gpar * 64:(gpar + 1) * 64, pl * 64:pl * 64 + nch], O.WS[:, g, ri, :],
                                                   Rb[:, g, 0:nch], start=True, stop=True), [kWS, kRb], [kk], "pe")

            def pv(pst):
                v = pst[:, :].rearrange("q (p c) -> q p c", c=64)[:, :, 0:nch]
                return rev_last(v) if rev else v
            ps_ = slice(sbk * 8, (sbk + 1) * 8)
            cs = O.COS[:, ps_, sl0:65]
            sn = O.SIN[:, ps_, sl0:65]
            a, b = ft1[:, :, 0:nch], ft2[:, :, 0:nch]
            V(lambda: nc.vector.tensor_tensor(a, pv(pre), cs, ALU.mult), [kre, kROT], ["ft1"])
            V(lambda: nc.vector.tensor_tensor(b, pv(pim), sn, ALU.mult), [kim, kROT], ["ft2"])
            V(lambda: nc.vector.tensor_tensor(Z[:, 0, ps_, sl0:65], a, b, ALU.add), ["ft1", "ft2"], ["Z"])
            V(lambda: nc.vector.tensor_tensor(a, pv(pim), cs, ALU.mult), [kim, kROT], ["ft1"])
            V(lambda: nc.vector.tensor_tensor(b, pv(pre), sn, ALU.mult), [kre, kROT], ["ft2"])
            V(lambda: nc.vector.tensor_tensor(Z[:, 1, ps_, sl0:65], a, b, ALU.subtract), ["ft1", "ft2"], ["Z"])
        for ri in range(2):
            V(lambda: nc.vector.tensor_tensor_scan(ZS[:, ri].rearrange("q p c -> q (p c)"),
                                                   O.MULT[:].rearrange("q p c -> q (p c)"),
                                                   Z[:, ri].rearrange("q p c -> q (p c)"), 0.0, ALU.mult, ALU.add),
              ["Z", kROT], ["ZS"])
        c64, s64 = O.COS[:, :, 64:65], O.SIN[:, :, 64:65]
        zr, zi = ZS[:, 0, :, 64:65], ZS[:, 1, :, 64:65]
        a, b = bt1[:, :, 0:1], bt2[:, :, 0:1]
        if want:
            rng_ = slice(sl0 - 1, 64)

            def sv(t):
                return rev_last(t) if rev else t
            hr, hi = sv(ZS[:, 0, :, rng_]), sv(ZS[:, 1, :, rng_])
            hc, hs = sv(O.COS[:, :, rng_]), sv(O.SIN[:, :, rng_])
            ha, hb = bt1[:, :, 0:nch], bt2[:, :, 0:nch]
            V(lambda: nc.vector.tensor_tensor(ha, hr, hc, ALU.mult), ["ZS", kROT], ["bt1"])
            V(lambda: nc.vector.tensor_tensor(hb, hi, hs, ALU.mult), ["ZS", kROT], ["bt2"])
            V(lambda: nc.vector.tensor_tensor(hist[:, 0, :, 0:nch], ha, hb, ALU.subtract), ["bt1", "bt2"], ["hist"])
            V(lambda: nc.vector.tensor_tensor(ha, hi, hc, ALU.mult), ["ZS", kROT], ["bt1"])
            V(lambda: nc.vector.tensor_tensor(hb, hr, hs, ALU.mult), ["ZS", kROT], ["bt2"])
            V(lambda: nc.vector.tensor_tensor(hist[:, 1, :, 0:nch], ha, hb, ALU.add), ["bt1", "bt2"], ["hist"])
        V(lambda: nc.vector.tensor_tensor(a, zr, c64, ALU.mult), ["ZS", kROT], ["bt1"])
        V(lambda: nc.vector.tensor_tensor(b, zi, s64, ALU.mult), ["ZS", kROT], ["bt2"])
        V(lambda: nc.vector.tensor_tensor(Z[:, 0, :, 0:1], a, b, ALU.subtract), ["bt1", "bt2"], ["Z"])
        V(lambda: nc.vector.tensor_tensor(a, zi, c64, ALU.mult), ["ZS", kROT], ["bt1"])
        V(lambda: nc.vector.tensor_tensor(b, zr, s64, ALU.mult), ["ZS", kROT], ["bt2"])
        V(lambda: nc.vector.tensor_tensor(Z[:, 1, :, 0:1], a, b, ALU.add), ["bt1", "bt2"], ["Z"])

    def back(bi):
        t0, nch, is_ctx, want = blocks[bi]
        if not want:
            return
        Rb, kRb = Rbs[bi % 2], "Rb%d" % (bi % 2)
        dom0 = t0 - OFF_HALO
        for gb in range(8):
            ydst = Sc.Y1[dom0:dom0 + 512, gb * 128:(gb + 1) * 128].rearrange("(c j) d -> c j d", j=8)
            kY1 = "Y1_%d_%d" % (dom0, gb)
            if dirn == 1:
                S.dma("sp", y1b[:], ydst, reads=[kY1], writes=["y1b"])
            for h4 in range(2):
                bank, bk = (K.ps[1], "ps1") if h4 == 0 else (K.ps[6], "ps6")
                for pq in range(2):
                    pair = gb * 4 + h4 * 2 + pq
                    o = bank[0:64, pq * 256:(pq + 1) * 256]
                    V(lambda: nc.tensor.matmul(o, hist[:, 0, pair, :], O.YS[:, pair, 0, :], start=True, stop=False),
                      ["hist", kYS], [bk], "pe")
                    V(lambda: nc.tensor.matmul(o, hist[:, 1, pair, :], O.YS[:, pair, 1, :], start=False, stop=False),
                      ["hist", kYS], [bk], "pe")
                    for gpar in range(2):
                        g = 2 * pair + gpar
                        V(lambda: nc.tensor.matmul(bank[0:64, pq * 256 + gpar * 128:pq * 256 + (gpar + 1) * 128],
                                                   Rb[:, g, :], O.Mop[:, g, :], start=False, stop=(gpar == 1)),
                          [kRb, kM], [bk], "pe")
                pin = bank[0:64, :].rearrange("c (g j h) -> c g j h", g=4, j=8)

                def dv(t):
                    return t[:, :, h4 * 64:(h4 + 1) * 64].rearrange("c j (g h) -> c g j h", g=4)
                if dirn == 0:
                    V(lambda: nc.scalar.copy(dv(ybuf), pin), [bk], ["ybuf"], "act")
                else:
                    V(lambda: nc.vector.tensor_tensor(dv(ybuf), pin, dv(y1b), ALU.add), [bk, "y1b"], ["ybuf"])
                    V(lambda: nc.scalar.activation(dv(ygb), dv(ybuf), AF.Gelu_apprx_tanh), ["ybuf"], ["ygb"], "act")
            if dirn == 0:
                S.dma("pool", ydst, ybuf[:], reads=["ybuf"], writes=[kY1])
            else:
                S.dma("pool", Sc.YG[dom0:dom0 + 512, gb * 128:(gb + 1) * 128].rearrange("(c j) d -> c j d", j=8), ygb[:],
                      reads=["ygb"], writes=["YG_%d" % dom0])

    front(0)
    for bi in range(len(blocks)):
        mid(bi)
        if bi + 1 < len(blocks):
            front(bi + 1)
        back(bi)


def tail_consts(K, I, Sc, st, l):
    nc, S = K.nc, K.S
    C = Ctx()
    sfx = "_t%d" % l
    C.sfx = sfx
    C.lng = bload(K, st, "lng" + sfx, I.ln_g[l, 0, :])
    C.lnb = bload(K, st, "lnb" + sfx, I.ln_b[l, 0, :])
    C.A2 = bload(K, st, "A2" + sfx, Sc.MOD[l, 0, 4 * D:5 * D], reads=["MOD"])
    C.B2 = bload(K, st, "B2" + sfx, Sc.MOD[l, 0, 3 * D:4 * D], reads=["MOD"])
    kA, kB = "A2" + sfx, "B2" + sfx
    S.op("dve", lambda: nc.vector.tensor_scalar_add(C.A2[:], C.A2[:], 1.0), reads=[kA], writes=[kA])
    S.op("dve", lambda: nc.vector.tensor_tensor(C.B2[:], C.lnb[:], C.A2[:], ALU.mult), reads=[kA, kB, "lnb" + sfx], writes=["B2tmp" + sfx, kB] if False else [kB])
    sh2 = bload(K, st, "sh2" + sfx, Sc.MOD[l, 0, 3 * D:4 * D], reads=["MOD"])
    S.op("dve", lambda: nc.vector.tensor_tensor(C.B2[:], C.B2[:], sh2[:], ALU.add), reads=[kB, "sh2" + sfx], writes=[kB])
    S.op("dve", lambda: nc.vector.tensor_tensor(C.A2[:], C.A2[:], C.lng[:], ALU.mult), reads=[kA, "lng" + sfx], writes=[kA])
    C.rw = st.enter_context(nc.sbuf_tensor("rw" + sfx, [128, 8, E], F32))
    S.dma("sp", C.rw[:], I.router_w.rearrange("(k p) e -> p k e", p=128), writes=["rw" + sfx])
    C.rb = bload(K, st, "rb" + sfx, I.router_b[:])
    C.keys = ["lng" + sfx, "lnb" + sfx, kA, kB, "rw" + sfx, "rb" + sfx]

    def sb(name, shape, dt=F32):
        return st.enter_context(nc.sbuf_tensor(name + sfx, list(shape), dt))
    C.par = []
    for pp in range(2):
        Wk = Ctx()
        Wk.sfx = sfx + "p%d" % pp
        sbp = lambda name, shape, dt=F32: sb(name + "p%d" % pp, shape, dt)
        Wk.st6 = sbp("st6", [128, 12])
        Wk.mv = sbp("mv", [128, 2])
        Wk.rstd = sbp("rstd", [128, 1])
        Wk.xn = sbp("xn", [128, D])
        Wk.x1 = sbp("x1t", [128, D])
        Wk.h2 = sbp("h2t", [128, D])
        Wk.h2Tb = sbp("h2Tb", [128, 8, 128], BF16)
        Wk.h2Tf = sbp("h2Tf", [128, 8, 128])
        Wk.r = {n: sbp("r_" + n, [128, 16]) for n in ("lg", "e", "mk1", "t", "e2", "mk2", "w", "cmb")}
        Wk.q = {n: sbp("q_" + n, [128, 4]) for n in ("m1", "m2", "gs", "gm", "rg")}
        Wk.s1 = {n: sbp("s_" + n, [128, 1]) for n in ("mx", "nmx", "gmax")}
        for a in ("lng", "lnb", "A2", "B2", "rw", "rb", "keys"):
            setattr(Wk, a, getattr(C, a))
        C.par.append(Wk)
    return C


def layer_norm_tile(K, C, z, zkey, lnsfx):
    nc, S = K.nc, K.S
    k = lambda n: n + lnsfx
    S.op("dve", lambda: nc.vector.bn_stats(C.st6[:, 0:6], z[:, 0:512]), reads=[zkey], writes=[k("st6")])
    S.op("dve", lambda: nc.vector.bn_stats(C.st6[:, 6:12], z[:, 512:1024]), reads=[zkey], writes=[k("st6")])
    S.op("dve", lambda: nc.vector.bn_aggr(C.mv[:], C.st6[:]), reads=[k("st6")], writes=[k("mv")])
    S.op("dve", lambda: nc.vector.tensor_scalar_add(C.rstd[:], C.mv[:, 1:2], EPS), reads=[k("mv")], writes=[k("rstd")])
    S.op("act", lambda: nc.scalar.sqrt(C.rstd[:], C.rstd[:]), reads=[k("rstd")], writes=[k("rstd")])
    S.op("dve", lambda: nc.vector.reciprocal(C.rstd[:], C.rstd[:]), reads=[k("rstd")], writes=[k("rstd")])
    S.op("dve", lambda: nc.vector.tensor_scalar(C.xn[:], z[:], C.mv[:, 0:1], C.rstd[:, 0:1], ALU.subtract, ALU.mult),
         reads=[zkey, k("mv"), k("rstd")], writes=[getattr(C, "xnkey", k("xn"))])


def tail_tile(K, I, Sc, C, l, z, zkey, t0):
    nc, S = K.nc, K.S
    C = C.par[(t0 // 128) % 2]
    pp = (t0 // 128) % 2
    sfx = C.sfx
    k = lambda n: n + sfx
    layer_norm_tile(K, C, z, zkey, sfx)
    S.op("pool", lambda: nc.gpsimd.tensor_tensor(C.x1[:], C.xn[:], C.lng[:], ALU.mult), reads=[k("xn")] + C.keys, writes=[k("x1")])
    S.op("pool", lambda: nc.gpsimd.tensor_tensor(C.x1[:], C.x1[:], C.lnb[:], ALU.add), reads=[k("x1")] + C.keys, writes=[k("x1")])
    S.dma("pool", Sc.X1[l][t0:t0 + 128, :], C.x1[:], reads=[k("x1")], writes=["X1_%d_%d" % (l, t0)])
    if GLUSUB < 4:
        return
    S.op("dve", lambda: nc.vector.tensor_tensor(C.h2[:], C.xn[:], C.A2[:], ALU.mult), reads=[k("xn")] + C.keys, writes=[k("h2")])
    S.op("dve", lambda: nc.vector.tensor_tensor(C.h2[:], C.h2[:], C.B2[:], ALU.add), reads=[k("h2")] + C.keys, writes=[k("h2")])
    for hb in range(2):
        bank, bk = K.ps[2 + hb + 3 * pp], "ps%d" % (2 + hb + 3 * pp)
        for q in range(4):
            kc = hb * 4 + q
            S.op("pe", lambda: nc.tensor.transpose(bank[:, q * 128:(q + 1) * 128], C.h2[:, kc * 128:(kc + 1) * 128], K.ident[:]),
                 reads=[k("h2"), "ident"], writes=[bk])
        S.op("dve", lambda: nc.vector.tensor_copy(C.h2Tf[:, hb * 4:(hb + 1) * 4, :].rearrange("p k t -> p (k t)"), bank[:, :]),
             reads=[bk], writes=[k("h2Tf")])
        S.op("act", lambda: nc.scalar.copy(C.h2Tb[:, hb * 4:(hb + 1) * 4, :].rearrange("p k t -> p (k t)"),
                                           C.h2Tf[:, hb * 4:(hb + 1) * 4, :].rearrange("p k t -> p (k t)")),
             reads=[k("h2Tf")], writes=[k("h2Tb")])
    for kc in range(8 if os.environ.get("NOH2T") is None else 0):
        S.dma("sp", Sc.H2T[l][kc, :, t0:t0 + 128], C.h2Tb[:, kc, :], reads=[k("h2Tb")], writes=["H2T_%d_%d" % (l, t0)])
    return


def tail_tile_B(K, I, Sc, C, l, t0):
    nc, S = K.nc, K.S
    C = C.par[(t0 // 128) % 2]
    sfx = C.sfx
    k = lambda n: n + sfx
    lgp, lk = K.ps[4], "ps4"
    for kc in range(8):
        S.op("pe", lambda: nc.tensor.matmul(lgp[:, 0:E], C.h2Tf[:, kc, :], C.rw[:, kc, :], start=(kc == 0), stop=(kc == 7)),
             reads=[k("h2Tf")] + C.keys, writes=[lk])
    r, q, s1 = C.r, C.q, C.s1
    kr = k("route")

    def V(fn, rd=(), eng="dve"):
        S.op(eng, fn, reads=[kr] + list(rd), writes=[kr])
    e3 = lambda t: t[:].rearrange("p (g i) -> p g i", i=4)
    b3 = lambda t: t[:].unsqueeze(2).to_broadcast([128, 4, 4])
    AXX = mybir.AxisListType.X
    V(lambda: nc.vector.tensor_tensor(r["lg"][:], lgp[:, 0:E], C.rb[:], ALU.add), [lk] + C.keys)
    V(lambda: nc.vector.tensor_reduce(s1["mx"][:], r["lg"][:], AXX, ALU.max))
    V(lambda: nc.vector.tensor_scalar_mul(s1["nmx"][:], s1["mx"][:], -1.0))
    V(lambda: nc.scalar.activation(r["e"][:], r["lg"][:], AF.Exp, bias=s1["nmx"][:, 0:1]), eng="act")
    V(lambda: nc.vector.tensor_reduce(q["m1"][:], e3(r["e"]), AXX, ALU.max))
    V(lambda: nc.vector.tensor_tensor(e3(r["mk1"]), e3(r["e"]), b3(q["m1"]), ALU.is_equal))
    V(lambda: nc.vector.tensor_tensor(r["t"][:], r["mk1"][:], r["e"][:], ALU.mult))
    V(lambda: nc.vector.tensor_tensor(r["e2"][:], r["e"][:], r["t"][:], ALU.subtract))
    V(lambda: nc.vector.tensor_reduce(q["m2"][:], e3(r["e2"]), AXX, ALU.max))
    V(lambda: nc.vector.tensor_tensor(e3(r["mk2"]), e3(r["e2"]), b3(q["m2"]), ALU.is_equal))
    V(lambda: nc.vector.tensor_tensor(q["gs"][:], q["m1"][:], q["m2"][:], ALU.add))
    V(lambda: nc.vector.tensor_reduce(s1["gmax"][:], q["gs"][:], AXX, ALU.max))
    V(lambda: nc.vector.tensor_tensor(q["gm"][:], q["gs"][:], s1["gmax"][:].to_broadcast([128, 4]), ALU.is_equal))
    V(lambda: nc.vector.reciprocal(q["rg"][:], q["gs"][:]))
    V(lambda: nc.vector.tensor_tensor(q["rg"][:], q["rg"][:], q["gm"][:], ALU.mult))
    V(lambda: nc.vector.tensor_tensor(r["w"][:], r["mk1"][:], r["mk2"][:], ALU.add))
    V(lambda: nc.vector.tensor_tensor(r["w"][:], r["w"][:], r["e"][:], ALU.mult))
    V(lambda: nc.vector.tensor_tensor(e3(r["cmb"]), e3(r["w"]), b3(q["rg"]), ALU.mult))
    S.dma("pool", Sc.CMB[l][t0:t0 + 128, :], r["cmb"][:], reads=[kr], writes=["CMB_%d_%d" % (l, t0)])


def phase_glu(K, I, Sc):
    nc, S = K.nc, K.S
    with ExitStack() as st:
        def sb(name, shape, dt=F32):
            return st.enter_context(nc.sbuf_tensor(name, list(shape), dt))
        C = tail_consts(K, I, Sc, st, 0)
        g1B = bload(K, st, "g1B", Sc.MOD[0, 0, 2 * D:3 * D], reads=["MOD"])
        wv = sb("wv", [128, 8, D], BF16)
        wg = sb("wgl", [128, 8, D], BF16)
        stage = [sb("gstage%d" % i, [128, D]) for i in range(2)]
        it = 0
        for wsrc, wdst, kn in ((I.w_val, wv, "wv"), (I.w_gate, wg, "wgl")):
            for kc in range(8):
                sg, sk = stage[it % 2], "gstage%d" % (it % 2)
                it += 1
                S.dma("sp", sg[:], wsrc[kc * 128:(kc + 1) * 128, :], writes=[sk])
                S.op("act", lambda: nc.scalar.copy(wdst[:, kc, :], sg[:]), reads=[sk], writes=[kn])
        dbuf = [[sb("ygt%d" % p, [128, D], BF16), sb("yT%d" % p, [128, 8, 128], BF16), sb("xt_g%d" % p, [128, D]),
                 sb("sgm%d" % p, [128, 512]), sb("mt%d" % p, [128, D]), sb("zt%d" % p, [128, D])] for p in range(3)]
        def glu_front(tt):
            t0 = tt * 128
            blk = (t0 // 512) * 512
            ygt, yT, xt, sgm, mt, zt = dbuf[tt % 3]
            kq = lambda n: n + "%d" % (tt % 3)
            S.dma("sp", ygt[:], Sc.YG[t0:t0 + 128, :], reads=["YG_%d" % blk], writes=[kq("ygt")])
            S.dma("sp", xt[:], I.seq[OFF_HALO + t0:OFF_HALO + t0 + 128, :], writes=[kq("xt_g")])
            for kc in range(8):
                S.op("pe", lambda: nc.tensor.transpose(K.psb[:, kc * 128:(kc + 1) * 128], ygt[:, kc * 128:(kc + 1) * 128], K.identb[:]),
                     reads=[kq("ygt"), "identb"], writes=["psb"])
            S.op("act", lambda: nc.scalar.copy(yT[:].rearrange("p k t -> p (k t)"), K.psb[:, :]), reads=["psb"], writes=[kq("yT")])
            for nh in range(2):
                for kc in range(8):
                    S.op("pe", lambda: nc.tensor.matmul(K.ps[0][:, :], yT[:, kc, :], wv[:, kc, nh * 512:(nh + 1) * 512],
                                                        start=(kc == 0), stop=(kc == 7)), reads=[kq("yT"), "wv"], writes=["ps0"])
                for kc in range(8):
                    S.op("pe", lambda: nc.tensor.matmul(K.ps[1][:, :], yT[:, kc, :], wg[:, kc, nh * 512:(nh + 1) * 512],
                                                        start=(kc == 0), stop=(kc == 7)), reads=[kq("yT"), "wgl"], writes=["ps1"])
                S.op("act", lambda: nc.scalar.activation(sgm[:], K.ps[1][:, :], AF.Sigmoid), reads=["ps1"], writes=[kq("sgm")])
                S.op("dve", lambda: nc.vector.tensor_tensor(mt[:, nh * 512:(nh + 1) * 512], K.ps[0][:, :], sgm[:], ALU.mult),
                     reads=["ps0", kq("sgm")], writes=[kq("mt")])
            S.op("dve", lambda: nc.vector.tensor_tensor(mt[:], mt[:], g1B[:], ALU.mult), reads=[kq("mt"), "g1B"], writes=[kq("mt")])
            S.op("dve", lambda: nc.vector.scalar_tensor_tensor(zt[:], xt[:], ALPHA, mt[:], ALU.mult, ALU.add),
                 reads=[kq("xt_g"), kq("mt")], writes=[kq("zt")])
            return zt, kq("zt"), t0
        NTL = NT0 // 128
        fq = [glu_front(0)]
        prev = None
        for tt in range(NTL):
            if tt + 1 < NTL:
                fq.append(glu_front(tt + 1))
            cur = fq.pop(0)
            tail_tile(K, I, Sc, C, 0, cur[0], cur[1], cur[2])
            if prev is not None:
                tail_tile_B(K, I, Sc, C, 0, prev)
            prev = cur[2]
        tail_tile_B(K, I, Sc, C, 0, prev)
        S.barrier()


def phase_moe(K, I, Sc, l, T, dst, dst_off):
    nc, S = K.nc, K.S
    TH = T // 2
    NTT = TH // 128
    chunks = []
    o = 0
    while o < TH:
        n = min(512, TH - o)
        chunks.append((o, n))
        o += n
    sfx = "_m%d" % l
    with ExitStack() as st:
        def sb(name, shape, dt=F32):
            return st.enter_context(nc.sbuf_tensor(name + sfx, list(shape), dt))
        g2B = bload(K, st, "g2B" + sfx, Sc.MOD[l, 0, 5 * D:6 * D], reads=["MOD"])
        lng = bload(K, st, "lng2" + sfx, I.ln_g[l, 1, :])
        lnb = bload(K, st, "lnb2" + sfx, I.ln_b[l, 1, :])
        ckeys = ["g2B" + sfx, "lng2" + sfx, "lnb2" + sfx]
        h2T = sb("h2T", [128, 8, TH], BF16)
        aT = sb("aT", [128, 8, TH], BF16)
        acc = sb("acc", [128, NTT, D])
        cmb = sb("cmb", [128, NTT, E])
        W = [[sb("w%d_%d" % (p, j), [128, 8, D], BF16) for j in range(3)] for p in range(2)]
        NSTG = 3
        stage = [sb("stage%d" % i, [128, D]) for i in range(NSTG)]
        sgt = [sb("sgt%d" % i, [128, 512]) for i in range(2)]
        L = Ctx()
        L.st6 = sb("st6", [128, 12]); L.mv = sb("mv", [128, 2]); L.rstd = sb("rstd", [128, 1])
        L.xn = stage[2]
        L.xnkey = "stage2" + sfx
        x1t, zt = stage[0], stage[1]
        kx1, kzt = "stage0" + sfx, "stage1" + sfx
        wsrc = (I.moe_wg, I.moe_wu, I.moe_wd)
        sidx = [0]

        def emit_piece(e, par, j, kc):
            i = sidx[0] % NSTG
            sidx[0] += 1
            sk = "stage%d" % i + sfx
            S.dma("sp", stage[i][:], wsrc[j][l, e, kc * 128:(kc + 1) * 128, :], writes=[sk])
            wk = "w%d_%d_%d" % (par, j, kc) + sfx
            sel = (j * 8 + kc) % 4
            if sel in (0, 2):
                S.op("act", lambda: nc.scalar.copy(W[par][j][:, kc, :], stage[i][:]), reads=[sk], writes=[wk])
            elif sel == 1:
                S.op("dve", lambda: nc.vector.tensor_copy(W[par][j][:, kc, :], stage[i][:]), reads=[sk], writes=[wk])
            else:
                S.op("pool", lambda: nc.gpsimd.tensor_copy(W[par][j][:, kc, :], stage[i][:]), reads=[sk], writes=[wk])

        def load_expert(e, par):
            for j in range(3):
                for kc in range(8):
                    emit_piece(e, par, j, kc)

        for half in range(2):
            hoff = half * TH
            hk = ["H2T_%d_%d" % (l, hoff + t * 128) for t in range(NTT)]
            S.dma("sp", h2T[:], Sc.H2T[l][:, :, hoff:hoff + TH].rearrange("k p t -> p k t"), reads=hk, writes=["h2T" + sfx])
            ck = ["CMB_%d_%d" % (l, hoff + t * 128) for t in range(NTT)]
            S.dma("sp", cmb[:], Sc.CMB[l][hoff:hoff + TH, :].rearrange("(t p) e -> p t e", p=128), reads=ck, writes=["cmb" + sfx])
            if half == 0:
                load_expert(0, 0)
            for e in range(E):
                par = e % 2
                if e + 1 < E:
                    pend = [(e + 1, 1 - par, j, kc) for j in range(3) for kc in range(8)]
                elif half == 0:
                    pend = [(0, 1 - par, j, kc) for j in range(3) for kc in range(8)]
                else:
                    pend = []
                wg_, wu_, wd_ = W[par]
                wkeys = lambda j: ["w%d_%d_%d" % (par, j, kc) + sfx for kc in range(8)]
                ci = 0
                for fc in range(8):
                    for (co, cn) in chunks:
                        pg, pu = K.ps[ci % 2], K.ps[2 + ci % 2]
                        kg, ku = "ps%d" % (ci % 2), "ps%d" % (2 + ci % 2)
                        sg_, sgk = sgt[ci % 2], "sgt%d" % (ci % 2) + sfx
                        ci += 1
                        for kc in range(8):
                            S.op("pe", lambda: nc.tensor.matmul(pg[:, 0:cn], wg_[:, kc, fc * 128:(fc + 1) * 128], h2T[:, kc, co:co + cn],
                                                                start=(kc == 0), stop=(kc == 7)),
                                 reads=["w%d_0_%d" % (par, kc) + sfx, "h2T" + sfx], writes=[kg])
                        for kc in range(8):
                            S.op("pe", lambda: nc.tensor.matmul(pu[:, 0:cn], wu_[:, kc, fc * 128:(fc + 1) * 128], h2T[:, kc, co:co + cn],
                                                                start=(kc == 0), stop=(kc == 7)),
                                 reads=["w%d_1_%d" % (par, kc) + sfx, "h2T" + sfx], writes=[ku])
                        S.op("act", lambda: nc.scalar.activation(sg_[:, 0:cn], pg[:, 0:cn], AF.Silu), reads=[kg], writes=[sgk])
                        S.op("dve", lambda: nc.vector.tensor_tensor(aT[:, fc, co:co + cn], sg_[:, 0:cn], pu[:, 0:cn], ALU.mult),
                             reads=[sgk, ku], writes=["aT%d" % fc + sfx])
                        for _ in range(2 if len(chunks) == 2 else 1):
                            if pend:
                                emit_piece(*pend.pop(0))
                while pend:
                    emit_piece(*pend.pop(0))
                di = 0
                for tt in range(NTT):
                    for dh in range(2):
                        pd, kd = K.ps[4 + di % 3], "ps%d" % (4 + di % 3)
                        di += 1
                        for fc in range(8):
                            S.op("pe", lambda: nc.tensor.matmul(pd[:, :], aT[:, fc, tt * 128:(tt + 1) * 128], wd_[:, fc, dh * 512:(dh + 1) * 512],
                                                                start=(fc == 0), stop=(fc == 7)),
                                 reads=["aT%d" % fc + sfx, "w%d_2_%d" % (par, fc) + sfx], writes=[kd])
                        ak = "acc%d" % tt + sfx
                        av = acc[:, tt, dh * 512:(dh + 1) * 512]
                        if e == 0:
                            S.op("dve", lambda: nc.vector.tensor_scalar(av, pd[:, :], cmb[:, tt, e:e + 1], None, ALU.mult),
                                 reads=[kd, "cmb" + sfx], writes=[ak])
                        else:
                            S.op("dve", lambda: nc.vector.scalar_tensor_tensor(av, pd[:, :], cmb[:, tt, e:e + 1], av, ALU.mult, ALU.add),
                                 reads=[kd, "cmb" + sfx, ak], writes=[ak])
            for tt in range(NTT):
                t0 = hoff + tt * 128
                ak = "acc%d" % tt + sfx
                S.dma("sp", x1t[:], Sc.X1[l][t0:t0 + 128, :], reads=["X1_%d_%d" % (l, t0)], writes=[kx1])
                S.op("pool", lambda: nc.gpsimd.tensor_tensor(acc[:, tt, :], acc[:, tt, :], g2B[:], ALU.mult), reads=[ak] + ckeys, writes=[ak])
                S.op("dve", lambda: nc.vector.scalar_tensor_tensor(zt[:], x1t[:], ALPHA, acc[:, tt, :], ALU.mult, ALU.add),
                     reads=[kx1, ak], writes=[kzt])
                layer_norm_tile(K, L, zt, kzt, sfx)
                S.op("pool", lambda: nc.gpsimd.tensor_tensor(zt[:], L.xn[:], lng[:], ALU.mult), reads=[L.xnkey] + ckeys, writes=[kzt])
                S.op("pool", lambda: nc.gpsimd.tensor_tensor(zt[:], zt[:], lnb[:], ALU.add), reads=[kzt] + ckeys, writes=[kzt])
                S.dma("pool", dst[dst_off + t0:dst_off + t0 + 128, :], zt[:], reads=[kzt], writes=["X2_%d_%d" % (l, t0)])
        S.barrier()


def phase_pool(K, I, Sc):
    nc, S = K.nc, K.S
    NR, NCOL = 40, 64
    PW = 84
    PR = 49
    with ExitStack() as st:
        def sb(name, shape, dt=F32):
            return st.enter_context(nc.sbuf_tensor(name + "_p", list(shape), dt))

        def V(fn, r, w, eng="dve"):
            S.op(eng, fn, reads=r, writes=w)
        C = tail_consts(K, I, Sc, st, 1)
        sc1 = sb("sc1col", [128, 8])
        S.dma("sp", sc1[:], Sc.MOD[1, 0, D:2 * D].rearrange("(k p) -> p k", p=128), reads=["MOD"], writes=["sc1col"],
              allow_slow_non_contiguous=True)
        V(lambda: nc.vector.tensor_scalar_add(sc1[:], sc1[:], 1.0), ["sc1col"], ["sc1col"])
        GS = bload(K, st, "GS_p", Sc.MOD[1, 0, 2 * D:3 * D], reads=["MOD"])
        psc = bload(K, st, "psc_p", I.pool_scale[:])
        V(lambda: nc.vector.tensor_tensor(GS[:], GS[:], psc[:], ALU.mult), ["GS_p", "psc_p"], ["GS_p"])
        INVC = bload(K, st, "INVC_p", I.invc.rearrange("a b -> (a b)"))
        INVR = bload(K, st, "INVR_p", I.invr.rearrange("a b -> (a b)"))
        SEL = bload(K, st, "SEL_p", I.sel[:])
        pwf = sb("pwf", [128, 4, 2, 256])
        pw = sb("pw", [128, 4, 2, 256], BF16)
        S.dma("sp", pwf[:], I.pool_w.rearrange("g (c p) d -> p g c d", p=128), writes=["pwf"])
        V(lambda: nc.scalar.copy(pw[:].rearrange("p g c d -> p (g c d)"), pwf[:].rearrange("p g c d -> p (g c d)")),
          ["pwf"], ["pw"], "act")
        pooledT = sb("pooledT", [128, 8, NOWN], BF16)
        inner = ExitStack()
        sbi = lambda name, shape, dt=F32: inner.enter_context(nc.sbuf_tensor(name + "_p", list(shape), dt))
        xcol = sbi("xcol", [128, NT0 // 128, 128])
        xp = sbi("xp", [128, PR, PW])
        A = sbi("pA", [128, PR, PW])
        B = sbi("pB", [128, PR, PW])
        Bc = sbi("pBc", [128, PR, NCOL])
        mean = sbi("pmean", [128, 32, NCOL])
        V(lambda: nc.gpsimd.memset(xp[:], 0.0), [], ["xp"], "pool")
        V(lambda: nc.gpsimd.memset(Bc[:], 0.0), [], ["pBc"], "pool")
        allx2 = ["X2_0_%d" % (t * 128) for t in range(NT0 // 128)]
        for dc in range(8):
            gk = dc // 2
            kk = POOLK[gk]
            steps = [s_ for s_ in (1, 2, 4, 8) if s_ < kk]
            S.dma("sp", xcol[:], Sc.X2[:, dc * 128:(dc + 1) * 128].rearrange("(t p) d -> p t d", p=128), reads=allx2, writes=["xcol"])
            for q in range(5):
                bank, bk = K.ps[q % 2], "ps%d" % (q % 2)
                for j in range(4):
                    V(lambda: nc.tensor.transpose(bank[:, j * 128:(j + 1) * 128], xcol[:, q * 4 + j, :], K.ident[:]),
                      ["xcol", "ident"], [bk], "pe")
                V(lambda: nc.scalar.copy(xp[:, q * 8:(q + 1) * 8, 8:8 + NCOL], bank[:, :].rearrange("p (r c) -> p r c", c=NCOL)),
                  [bk], ["xp"], "act")
            cur, ck = xp, "xp"
            W = PW
            bufs = [(A, "pA"), (B, "pB")]
            for si, s_ in enumerate(steps):
                nb, nk = bufs[si % 2]
                V(lambda: nc.vector.tensor_tensor(nb[:, 0:NR, 0:W - s_], cur[:, 0:NR, 0:W - s_], cur[:, 0:NR, s_:W], ALU.add),
                  [ck], [nk])
                cur, ck = nb, nk
                W -= s_
            o0 = 8 - kk // 2
            V(lambda: nc.vector.tensor_scalar(Bc[:, 0:NR, :], cur[:, 0:NR, o0:o0 + NCOL], SEL[:, 0:1], None, ALU.mult),
              [ck, "SEL_p"], ["pBc"])
            V(lambda: nc.vector.scalar_tensor_tensor(Bc[:, 0:NR, :], cur[:, 0:NR, o0 + 1:o0 + 1 + NCOL], SEL[:, 1:2], Bc[:, 0:NR, :],
                                                     ALU.mult, ALU.add), [ck, "SEL_p", "pBc"], ["pBc"])
            V(lambda: nc.vector.tensor_tensor(Bc[:, 0:NR, :], Bc[:, 0:NR, :],
                                              INVC[:, gk * 64:(gk + 1) * 64].unsqueeze(1).to_broadcast([128, NR, NCOL]), ALU.mult),
              ["pBc", "INVC_p"], ["pBc"])
            cur, ck = Bc, "pBc"
            Hh = PR
            bufs = [(A, "pA"), (B, "pB")]
            for si, s_ in enumerate(steps):
                nb, nk = bufs[si % 2]
                V(lambda: nc.vector.tensor_tensor(nb[:, 0:Hh - s_, 0:NCOL], cur[:, 0:Hh - s_, 0:NCOL], cur[:, s_:Hh, 0:NCOL], ALU.add),
                  [ck], [nk])
                cur, ck = nb, nk
                Hh -= s_
            r0 = 8 - kk // 2
            V(lambda: nc.vector.tensor_scalar(mean[:], cur[:, r0:r0 + 32, 0:NCOL], SEL[:, 0:1], None, ALU.mult), [ck, "SEL_p"], ["pmean"])
            V(lambda: nc.vector.scalar_tensor_tensor(mean[:], cur[:, r0 + 1:r0 + 33, 0:NCOL], SEL[:, 1:2], mean[:], ALU.mult, ALU.add),
              [ck, "SEL_p", "pmean"], ["pmean"])
            V(lambda: nc.vector.tensor_tensor(mean[:], mean[:],
                                              INVR[:, gk * 32:(gk + 1) * 32].unsqueeze(2).to_broadcast([128, 32, NCOL]), ALU.mult),
              ["pmean", "INVR_p"], ["pmean"])
            V(lambda: nc.vector.tensor_tensor(mean[:], mean[:], xp[:, 8:40, 8:8 + NCOL], ALU.subtract), ["pmean", "xp"], ["pmean"])
            V(lambda: nc.scalar.activation(pooledT[:, dc, :].rearrange("p (r c) -> p r c", c=NCOL), mean[:], AF.Identity,
                                           scale=sc1[:, dc:dc + 1]), ["pmean", "sc1col"], ["pooledT%d" % dc], "act")
        S.barrier()
        inner.close()
        pbuf = [[sb("x2t%d" % p, [128, D]), sb("mtp%d" % p, [128, D]), sb("ztp%d" % p, [128, D])] for p in range(3)]
        def pool_front(tt):
            t0 = tt * 128
            x2t, mt, zt = pbuf[tt % 3]
            kq = lambda n: n + "%d" % (tt % 3)
            S.dma("sp", x2t[:], Sc.X2[NHALO + t0:NHALO + t0 + 128, :], reads=["X2_0_%d" % (NHALO + t0)], writes=[kq("x2t_p")])
            for gk in range(4):
                bank, bk = K.ps[gk // 2], "ps%d" % (gk // 2)
                for cc in range(2):
                    V(lambda: nc.tensor.matmul(bank[:, (gk % 2) * 256:(gk % 2 + 1) * 256], pooledT[:, 2 * gk + cc, t0:t0 + 128],
                                               pw[:, gk, cc, :], start=(cc == 0), stop=(cc == 1)),
                      ["pooledT%d" % (2 * gk + cc), "pw"], [bk], "pe")
            for hb in range(2):
                V(lambda: nc.vector.tensor_tensor(mt[:, hb * 512:(hb + 1) * 512], K.ps[hb][:, :], GS[:, hb * 512:(hb + 1) * 512], ALU.mult),
                  ["ps%d" % hb, "GS_p"], [kq("mtp")])
            V(lambda: nc.vector.scalar_tensor_tensor(zt[:], x2t[:], ALPHA, mt[:], ALU.mult, ALU.add), [kq("x2t_p"), kq("mtp")], [kq("ztp")])
            return zt, kq("ztp"), t0
        NTL = NOWN // 128
        fq = [pool_front(0)]
        prev = None
        for tt in range(NTL):
            if tt + 1 < NTL:
                fq.append(pool_front(tt + 1))
            cur = fq.pop(0)
            tail_tile(K, I, Sc, C, 1, cur[0], cur[1], cur[2])
            if prev is not None:
                tail_tile_B(K, I, Sc, C, 1, prev)
            prev = cur[2]
        tail_tile_B(K, I, Sc, C, 1, prev)
        S.barrier()
```

```python
from contextlib import ExitStack
import math
import numpy as np
import concourse.bass as bass
import concourse.mybir as mybir
from concourse.bass_utils import run_bass_kernel_spmd

F32 = mybir.dt.float32
BF16 = mybir.dt.bfloat16
I32 = mybir.dt.int32
ALU = mybir.AluOpType
AF = mybir.ActivationFunctionType

D = 1024
NCTX = 256
NOWN = 2048
NHALO = 512
NT0 = NOWN + NHALO
SEQLEN = NCTX + 2 * NOWN + NCTX
OFF_HALO = NCTX + NOWN - NHALO
OFF_OWN = NCTX + NOWN
OFF_CTX1 = NCTX + 2 * NOWN
G, P_, H = 64, 64, 16
E = 16
ALPHA = 4.0 ** 0.25
EPS = 1e-5
POOLK = (2, 4, 8, 16)
TWO_PI = 2.0 * math.pi

DEBUG = False
STAGE = 99
import os
GLUSUB = int(os.environ.get("GLUSUB", "9"))

ENGS = ("pe", "act", "dve", "pool")
NDMA = 4
EPOCH = 24000


class Sched:
    def __init__(self, nc, stack):
        self.nc = nc
        self.stack = stack
        self.eng = {"pe": nc.tensor, "act": nc.scalar, "dve": nc.vector, "pool": nc.gpsimd, "sp": nc.sync}
        self.sem = {}
        self.cnt = {}
        self.epoch = {}
        for e in ENGS:
            self.epoch[e] = 0
            self.sem[(e, 0)] = stack.enter_context(nc.semaphore("s_%s0" % e))
            self.cnt[e] = 0
        self.dsem, self.dcnt, self.dring = {}, {}, {}
        for q in ("sp", "act", "pool"):
            self.dsem[q] = [stack.enter_context(nc.semaphore("d_%s%d" % (q, i))) for i in range(NDMA)]
            self.dcnt[q] = [0] * NDMA
            self.dring[q] = 0
        self.seen = {e: {} for e in self.eng}
        self.lastw = {}
        self.readers = {}
        self.nwaits = 0
        self.nops = 0

    def _semobj(self, key):
        return self.sem[(key[1], key[2])] if key[0] == "e" else self.dsem[key[1]][key[2]]

    def _wait(self, eng, tok):
        if tok is None:
            return
        key, val = tok
        if eng == "pe" and key[0] == "e" and key[1] == "pe":
            return
        if self.seen[eng].get(key, 0) >= val:
            return
        self.eng[eng].wait_ge(self._semobj(key), val)
        self.seen[eng][key] = val
        self.nwaits += 1

    def _deps(self, eng, reads, writes):
        for k in reads:
            self._wait(eng, self.lastw.get(k))
        for k in writes:
            self._wait(eng, self.lastw.get(k))
            for t in self.readers.get(k, {}).values():
                self._wait(eng, t)

    def _commit(self, tok, reads, writes):
        for k in reads:
            self.readers.setdefault(k, {})[tok[0]] = tok
        for k in writes:
            self.lastw[k] = tok
            self.readers[k] = {}

    def op(self, eng, fn, reads=(), writes=()):
        self._deps(eng, reads, writes)
        if self.cnt[eng] >= EPOCH:
            self.epoch[eng] += 1
            self.cnt[eng] = 0
            self.sem[(eng, self.epoch[eng])] = self.stack.enter_context(
                self.nc.semaphore("s_%s%d" % (eng, self.epoch[eng])))
        ins = fn()
        self.cnt[eng] += 1
        ep = self.epoch[eng]
        ins.then_inc(self.sem[(eng, ep)], 1)
        tok = (("e", eng, ep), self.cnt[eng])
        self._commit(tok, reads, writes)
        self.nops += 1
        return tok

    def dma(self, q, out, in_, reads=(), writes=(), **kw):
        self._deps(q, reads, writes)
        i = self.dring[q]
        self.dring[q] = (i + 1) % NDMA
        key = ("d", q, i)
        if self.dcnt[q][i] > 0:
            self._wait(q, (key, self.dcnt[q][i]))
        ins = self.eng[q].dma_start(out=out, in_=in_, **kw)
        self.dcnt[q][i] += 16
        ins.then_inc(self.dsem[q][i], 16)
        tok = (key, self.dcnt[q][i])
        self._commit(tok, reads, writes)
        return tok

    def barrier(self):
        for e in ("pe", "act", "dve", "pool", "sp"):
            self.wait_all(e)

    def wait_all(self, eng):
        for e in ENGS:
            if self.cnt[e] > 0:
                self._wait(eng, (("e", e, self.epoch[e]), self.cnt[e]))
        for q in self.dsem:
            for i in range(NDMA):
                if self.dcnt[q][i] > 0:
                    self._wait(eng, (("d", q, i), self.dcnt[q][i]))


def rev_last(ap):
    pat = [list(x) for x in ap.ap]
    step, cnt = pat[-1]
    off = ap.offset + step * (cnt - 1)
    pat[-1] = [-step, cnt]
    return bass.AP(ap.tensor, off, pat)


class Ctx:
    pass


def build_program():
    nc = bass.Bass("TRN2", target_bir_lowering=False)
    K = Ctx()
    K.nc = nc

    def din(name, shape, dt=F32):
        return nc.dram_tensor(name, list(shape), dt, kind="ExternalInput").ap()

    def dscr(name, shape, dt=F32, dbg=False):
        kind = "ExternalOutput"
        return nc.dram_tensor(name, list(shape), dt, kind=kind).ap()

    I = Ctx()
    I.seq = din("seq", [SEQLEN, D])
    I.cvec = din("cvec", [2, D])
    I.mod_w = din("mod_w", [2, D, 6 * D])
    I.mod_b = din("mod_b", [2, 6 * D])
    I.ln_g = din("ln_g", [2, 2, D])
    I.ln_b = din("ln_b", [2, 2, D])
    I.lam_re = din("lam_re", [2, G, P_])
    I.lam_im = din("lam_im", [2, G, P_])
    I.log_dt = din("log_dt", [2, G])
    I.b_re = din("b_re", [2, G, P_, H])
    I.b_im = din("b_im", [2, G, P_, H])
    I.c_re = din("c_re", [2, G, H, P_])
    I.c_im = din("c_im", [2, G, H, P_])
    I.s5_d = din("s5_d", [D])
    I.w_val = din("w_val", [D, D])
    I.w_gate = din("w_gate", [D, D])
    I.pool_w = din("pool_w", [4, 256, 256])
    I.pool_scale = din("pool_scale", [D])
    I.router_w = din("router_w", [D, E])
    I.router_b = din("router_b", [E])
    if STAGE >= 3:
        I.moe_wg = din("moe_wg", [2, E, D, D])
        I.moe_wu = din("moe_wu", [2, E, D, D])
        I.moe_wd = din("moe_wd", [2, E, D, D])
    I.ident = din("ident", [128, 128])
    I.maskp = din("maskp", [128, 128])
    I.maskq = din("maskq", [128, 128])
    I.invc = din("invc", [4, 64])
    I.invr = din("invr", [4, 32])
    I.sel = din("sel", [2])
    out = nc.dram_tensor("out", [NOWN, D], F32, kind="ExternalOutput").ap()

    Sc = Ctx()
    Sc.MOD = dscr("MOD", [2, 2, 6 * D], dbg=True)
    Sc.Y1 = dscr("Y1", [NT0, D])
    Sc.YG = dscr("YG", [NT0, D], BF16, dbg=True)
    Sc.X1 = [dscr("X1_0", [NT0, D], dbg=True), dscr("X1_1", [NOWN, D], dbg=True)]
    Sc.H2T = [dscr("H2T_0", [8, 128, NT0], BF16), dscr("H2T_1", [8, 128, NOWN], BF16)]
    Sc.CMB = [dscr("CMB_0", [NT0, E], dbg=True), dscr("CMB_1", [NOWN, E], dbg=True)]
    Sc.X2 = dscr("X2", [NT0, D], dbg=True)

    with ExitStack() as top:
        S = Sched(nc, top)
        K.S = S
        K.ps = [top.enter_context(nc.psum_tensor("ps%d" % i, [128, 512], F32)) for i in range(7)]
        K.psb = top.enter_context(nc.psum_tensor("psb", [128, 1024], BF16))
        K.psf = K.psb[:, :].bitcast(F32)
        K.ident = top.enter_context(nc.sbuf_tensor("identS", [128, 128], F32))
        K.identb = top.enter_context(nc.sbuf_tensor("identbS", [128, 128], BF16))
        S.dma("sp", K.ident[:], I.ident[:, :], writes=["ident"])
        S.op("dve", lambda: nc.vector.tensor_copy(K.identb[:], K.ident[:]), reads=["ident"], writes=["identb"])

        if STAGE >= 0:
            phase_mod(K, I, Sc)
        if STAGE >= 1:
            phase_s5(K, I, Sc)
        if STAGE >= 2:
            phase_glu(K, I, Sc)
        if STAGE >= 3:
            phase_moe(K, I, Sc, 0, NT0, Sc.X2, 0)
        if STAGE >= 4:
            phase_pool(K, I, Sc)
        if STAGE >= 5:
            phase_moe(K, I, Sc, 1, NOWN, out, 0)
        else:
            with ExitStack() as st:
                z = st.enter_context(nc.sbuf_tensor("zz", [128, D], F32))
                S.op("dve", lambda: nc.vector.memset(z[:], 0.0), writes=["zz"])
                for t in range(NOWN // 128):
                    S.dma("sp", out[t * 128:(t + 1) * 128, :], z[:], reads=["zz"])
        S.wait_all("sp")
        K.stats = (S.nops, S.nwaits)
    return nc


def bcast_load(K, st, name, src_row):
    nc, S = K.nc, K.S
    n = src_row.shape[-1]
    t = st.enter_context(nc.sbuf_tensor(name, [128, n], F32))
    S.dma("sp", t[:], src_row.partition_broadcast(128), writes=[name])
    return t


def dbg_dump(K, name, ap, reads):
    if not DEBUG:
        return
    nc, S = K.nc, K.S
    t = nc.dram_tensor("DBG_" + name, list(ap.shape), ap.dtype, kind="ExternalOutput").ap()
    S.dma("sp", t, ap, reads=list(reads))


def bload(K, st, name, src_row, reads=()):
    nc, S = K.nc, K.S
    n = src_row.shape[-1]
    t = st.enter_context(nc.sbuf_tensor(name, [128, n], F32))
    S.dma("sp", t[:], src_row.partition_broadcast(128), reads=list(reads), writes=[name])
    return t


def phase_mod(K, I, Sc):
    nc, S = K.nc, K.S
    with ExitStack() as st:
        cT = st.enter_context(nc.sbuf_tensor("cT", [128, 8, 2], F32))
        sT = st.enter_context(nc.sbuf_tensor("sT", [128, 8, 2], F32))
        for r in range(2):
            S.dma("sp", cT[:, :, r], I.cvec[r, :].rearrange("(k p) -> p k", p=128), writes=["cT"],
                  allow_slow_non_contiguous=True)
        S.op("act", lambda: nc.scalar.activation(sT[:], cT[:], AF.Silu), reads=["cT"], writes=["sT"])
        wb = [st.enter_context(nc.sbuf_tensor("modw%d" % i, [128, 3072], F32)) for i in range(2)]
        mb = st.enter_context(nc.sbuf_tensor("modb", [2, 3072], F32))
        mrow = st.enter_context(nc.sbuf_tensor("mrow", [2, 3072], F32))
        it = 0
        for l in range(2):
            for cg in range(2):
                S.dma("sp", mb[:], I.mod_b[l, cg * 3072:(cg + 1) * 3072].partition_broadcast(2), writes=["modb"])
                for kc in range(8):
                    w = wb[it % 2]
                    wk = "modw%d" % (it % 2)
                    it += 1
                    S.dma("sp" if kc % 2 == 0 else "act", w[:], I.mod_w[l, kc * 128:(kc + 1) * 128, cg * 3072:(cg + 1) * 3072],
                          writes=[wk])
                    for cb in range(6):
                        S.op("pe", lambda: nc.tensor.matmul(K.ps[cb][0:2, :], sT[:, kc, :], w[:, cb * 512:(cb + 1) * 512],
                                                            start=(kc == 0), stop=(kc == 7)),
                             reads=[wk, "sT"], writes=["ps%d" % cb])
                for cb in range(6):
                    S.op("dve", lambda: nc.vector.tensor_tensor(mrow[:, cb * 512:(cb + 1) * 512], K.ps[cb][0:2, :],
                                                                mb[:, cb * 512:(cb + 1) * 512], ALU.add),
                         reads=["ps%d" % cb, "modb"], writes=["mrow"])
                S.dma("sp", Sc.MOD[l, :, cg * 3072:(cg + 1) * 3072], mrow[:], reads=["mrow"], writes=["MOD"])
        S.barrier()


def _box_counts(n, k, lo_shift):
    t = np.arange(n)
    lo = np.clip(t - k // 2, 0, n)
    hi = np.clip(t + k - k // 2, 0, n)
    return (hi - lo).astype(np.float32)


def make_in_maps(inp):
    f32 = np.float32
    x = np.asarray(inp["x"], f32)
    c = np.asarray(inp["c"], f32)
    ctx = np.asarray(inp["ctx"], f32)
    c_ctx = np.asarray(inp["c_ctx"], f32)
    ident = np.eye(128, dtype=f32)
    s_idx = np.arange(128) // 16
    maskp = (s_idx[:, None] <= s_idx[None, :]).astype(f32)
    maskq = (s_idx[:, None] >= s_idx[None, :]).astype(f32)
    shared = {
        "mod_w": np.ascontiguousarray(inp["mod_w"], f32), "mod_b": np.ascontiguousarray(inp["mod_b"], f32),
        "ln_g": np.ascontiguousarray(inp["ln_g"], f32), "ln_b": np.ascontiguousarray(inp["ln_b"], f32),
        "s5_d": np.ascontiguousarray(inp["s5_d"][0], f32),
        "w_val": np.ascontiguousarray(inp["s5_w_val"][0], f32), "w_gate": np.ascontiguousarray(inp["s5_w_gate"][0], f32),
        "pool_w": np.ascontiguousarray(inp["pool_w"][0], f32), "pool_scale": np.ascontiguousarray(inp["pool_scale"][0], f32),
        "router_w": np.ascontiguousarray(inp["router_w"], f32), "router_b": np.ascontiguousarray(inp["router_b"], f32),
        "moe_wg": np.ascontiguousarray(inp["moe_w_gate"], f32), "moe_wu": np.ascontiguousarray(inp["moe_w_up"], f32),
        "moe_wd": np.ascontiguousarray(inp["moe_w_down"], f32),
        "ident": ident, "maskp": maskp, "maskq": maskq,
    }
    maps = []
    for k in range(8):
        b, half = k // 2, k % 2
        if half == 1:
            own, oth, cf = x[b, NOWN:], x[b, :NOWN], ctx[b]
            dirs = [0, 1]
        else:
            own, oth, cf = x[b, :NOWN][::-1], x[b, NOWN:][::-1], ctx[b][::-1]
            dirs = [1, 0]
        m = dict(shared)
        m["seq"] = np.ascontiguousarray(np.concatenate([cf, oth, own, cf], axis=0))
        m["cvec"] = np.ascontiguousarray(np.stack([c[b], c_ctx]))
        for nm, src in (("lam_re", "s5_lam_re"), ("lam_im", "s5_lam_im"), ("log_dt", "s5_log_dt"), ("b_re", "s5_b_re"),
                        ("b_im", "s5_b_im"), ("c_re", "s5_c_re"), ("c_im", "s5_c_im")):
            m[nm] = np.ascontiguousarray(np.asarray(inp[src], f32)[0][dirs])
        invc = np.zeros((4, 64), f32)
        invr = np.zeros((4, 32), f32)
        for gi, kk in enumerate(POOLK):
            cc = _box_counts(64, kk, 0)
            if half == 1:
                invc[gi] = 1.0 / cc
                invr[gi] = 1.0 / cc[32:64]
            else:
                invc[gi] = 1.0 / cc[::-1]
                invr[gi] = 1.0 / cc[0:32][::-1]
        m["invc"], m["invr"] = invc, invr
        m["sel"] = np.array([1.0, 0.0] if half == 1 else [0.0, 1.0], f32)
        maps.append(m)
    return maps


_CACHE = {}


def kernel(**inputs):
    key = (DEBUG, STAGE)
    if key not in _CACHE:
        _CACHE[key] = build_program()
    nc = _CACHE[key]
    maps = make_in_maps(inputs)
    names = set()
    for alloc in nc.allocations:
        try:
            if alloc.kind == "ExternalInput":
                names.add(alloc.memorylocations[0].name)
        except Exception:
            pass
    if names:
        maps = [{k: v for k, v in m.items() if k in names} for m in maps]
    res = run_bass_kernel_spmd(nc, maps, core_ids=list(range(8)))
    kernel.last = res
    outp = np.zeros((4, 2 * NOWN, D), np.float32)
    for k in range(8):
        b, half = k // 2, k % 2
        o = res.results[k]["out"]
        if half == 1:
            outp[b, NOWN:] = o
        else:
            outp[b, :NOWN] = o[::-1]
    return outp


def phase_s5(K, I, Sc):
    for dirn in (0, 1):
        with ExitStack() as st:
            O = s5_build_ops(K, I, Sc, st, dirn)
            s5_pass(K, I, Sc, st, dirn, O)
            K.S.barrier()


def s5_build_ops(K, I, Sc, st, dirn):
    nc, S = K.nc, K.S
    sfx = "_%d" % dirn
    O = Ctx()

    def sb(stack, name, shape, dt=F32):
        return stack.enter_context(nc.sbuf_tensor(name + sfx, list(shape), dt))

    O.WS = sb(st, "WS", [128, G, 2, 64], BF16)
    O.Mop = sb(st, "Mop", [128, G, 128], BF16)
    O.YS = sb(st, "YS", [128, 32, 2, 256], BF16)
    O.COS = sb(st, "COS", [128, 32, 65])
    O.SIN = sb(st, "SIN", [128, 32, 65])
    O.MULT = sb(st, "MULT", [128, 32, 65])
    O.tab = sb(st, "tab", [128, 5, G])
    kWS, kM, kYS, kROT, kTAB = "WS" + sfx, "Mop" + sfx, "YS" + sfx, "ROT" + sfx, "tab" + sfx

    def V(fn, r, w, eng="dve"):
        S.op(eng, fn, reads=r, writes=w)

    with ExitStack() as tt:
        rows = [Sc.MOD[0, 0, D:2 * D], Sc.MOD[0, 0, 0:D], Sc.MOD[0, 1, D:2 * D], Sc.MOD[0, 1, 0:D], I.s5_d[:]]
        for i, row in enumerate(rows):
            S.dma("sp", O.tab[0:16, i, :], row.rearrange("(g h) -> h g", h=16), reads=["MOD"], writes=[kTAB],
                  allow_slow_non_contiguous=True)
        for s_ in range(1, 8):
            S.dma("sp", O.tab[s_ * 16:(s_ + 1) * 16, :, :], O.tab[0:16, :, :], reads=[kTAB], writes=[kTAB + "r%d" % s_])
        allt = [kTAB] + [kTAB + "r%d" % s_ for s_ in range(1, 8)]
        for i in (0, 2):
            V(lambda: nc.vector.tensor_scalar_add(O.tab[:, i, :], O.tab[:, i, :], 1.0), allt, allt)

        LT = sb(tt, "LT", [64, 2, 128])
        lamA = sb(tt, "lamA", [128, 2, G])
        ldt = sb(tt, "ldt", [128, G])
        BA = sb(tt, "BA", [128, 2, G, H])
        CT = sb(tt, "CT", [128, 2, 8, 128])
        CA = sb(tt, "CA", [128, 2, G, H])
        for ri, arr in ((0, I.lam_re), (1, I.lam_im)):
            for dup in range(2):
                S.dma("sp", LT[:, ri, dup * 64:(dup + 1) * 64], arr[dirn, :, :], writes=["LT" + sfx])
        S.dma("sp", ldt[:], I.log_dt[dirn, :].partition_broadcast(128), writes=["ldt" + sfx])
        for ri, arr in ((0, I.b_re), (1, I.b_im)):
            for dup in range(2):
                S.dma("act", BA[dup * 64:(dup + 1) * 64, ri, :, :], arr[dirn].rearrange("g p h -> p g h"),
                      writes=["BA" + sfx])
        for ri, arr in ((0, I.c_re), (1, I.c_im)):
            for dup in range(2):
                S.dma("act", CT[:, ri, :, dup * 64:(dup + 1) * 64],
                      arr[dirn].rearrange("(gb gl) h p -> (gl h) gb p", gl=8), writes=["CT" + sfx])
        for ri in range(2):
            V(lambda: nc.tensor.transpose(K.ps[0][:, ri * 64:(ri + 1) * 64], LT[:, ri, :], K.ident[0:64, 0:64]),
              ["LT" + sfx, "ident"], ["ps0"], "pe")
        V(lambda: nc.vector.tensor_copy(lamA[:].rearrange("q r g -> q (r g)"), K.ps[0][:, 0:128]), ["ps0"], ["lamA" + sfx])
        for ri in range(2):
            for gq in range(2):
                for gb4 in range(4):
                    gb = gq * 4 + gb4
                    V(lambda: nc.tensor.transpose(K.ps[1 + gq][:, gb4 * 128:(gb4 + 1) * 128], CT[:, ri, gb, :], K.ident[:]),
                      ["CT" + sfx, "ident"], ["ps%d" % (1 + gq)], "pe")
                V(lambda: nc.vector.tensor_copy(CA[:, ri, gq * 32:(gq + 1) * 32, :].rearrange("q g h -> q (g h)"),
                                                K.ps[1 + gq][:, :]), ["ps%d" % (1 + gq)], ["CA" + sfx])

        sm = {}

        def T(name):
            if name not in sm:
                sm[name] = sb(tt, "sm_" + name, [128, G])
            return sm[name]

        kS = "small" + sfx

        def tt_(o, a, b, op):
            V(lambda: nc.vector.tensor_tensor(o, a, b, op), [kS, "lamA" + sfx, "ldt" + sfx], [kS])

        def ts_(o, a, s1, op):
            V(lambda: nc.vector.tensor_single_scalar(o, a, s1, op), [kS], [kS])

        lr, li = lamA[:, 0, :], lamA[:, 1, :]
        dtA = T("dt")
        V(lambda: nc.scalar.activation(dtA[:], ldt[:], AF.Exp), ["ldt" + sfx], [kS], "act")
        tt_(T("th")[:], li, dtA[:], ALU.mult)
        tt_(T("lrdt")[:], lr, dtA[:], ALU.mult)
        V(lambda: nc.scalar.activation(T("magP")[:], T("lrdt")[:], AF.Exp), [kS], [kS], "act")
        V(lambda: nc.scalar.activation(T("magN")[:], T("lrdt")[:], AF.Exp, scale=-1.0), [kS], [kS], "act")
        V(lambda: nc.scalar.activation(T("mag8")[:], T("lrdt")[:], AF.Exp, scale=8.0), [kS], [kS], "act")
        ts_(T("y")[:], T("th")[:], 1.0 / TWO_PI, ALU.mult)
        ni = tt.enter_context(nc.sbuf_tensor("ni" + sfx, [128, G], I32))
        for off, dst in ((0.0, "sin"), (0.25, "cos")):
            ts_(T("y2")[:], T("y")[:], off, ALU.add)
            V(lambda: nc.vector.tensor_copy(ni[:], T("y2")[:]), [kS], [kS])
            V(lambda: nc.vector.tensor_copy(T("nf")[:], ni[:]), [kS], [kS])
            tt_(T("r")[:], T("y2")[:], T("nf")[:], ALU.subtract)
            ts_(T("m")[:], T("r")[:], 0.5, ALU.is_gt)
            tt_(T("r")[:], T("r")[:], T("m")[:], ALU.subtract)
            ts_(T("m")[:], T("r")[:], -0.5, ALU.is_lt)
            tt_(T("r")[:], T("r")[:], T("m")[:], ALU.add)
            V(lambda: nc.scalar.activation(T(dst)[:], T("r")[:], AF.Sin, scale=6.28318), [kS], [kS], "act")

        PF = sb(tt, "PF", [128, 2, 9, G])
        PN = sb(tt, "PN", [128, 2, 8, G])

        def cmul(ore, oim, are, aim, bre, bim, t1, t2, r, w):
            V(lambda: nc.vector.tensor_tensor(t1, are, bre, ALU.mult), r, w)
            V(lambda: nc.vector.tensor_tensor(t2, aim, bim, ALU.mult), r, w)
            V(lambda: nc.vector.tensor_tensor(ore, t1, t2, ALU.subtract), r, w)
            V(lambda: nc.vector.tensor_tensor(t1, are, bim, ALU.mult), r, w)
            V(lambda: nc.vector.tensor_tensor(t2, aim, bre, ALU.mult), r, w)
            V(lambda: nc.vector.tensor_tensor(oim, t1, t2, ALU.add), r, w)

        for tab in (PF, PN):
            V(lambda: nc.vector.memset(tab[:, 0, 0, :], 1.0), [kS], [kS])
            V(lambda: nc.vector.memset(tab[:, 1, 0, :], 0.0), [kS], [kS])
        tt_(PF[:, 0, 1, :], T("magP")[:], T("cos")[:], ALU.mult)
        tt_(PF[:, 1, 1, :], T("magP")[:], T("sin")[:], ALU.mult)
        tt_(PN[:, 0, 1, :], T("magN")[:], T("cos")[:], ALU.mult)
        tt_(T("m")[:], T("magN")[:], T("sin")[:], ALU.mult)
        ts_(PN[:, 1, 1, :], T("m")[:], -1.0, ALU.mult)
        for k in range(2, 9):
            cmul(PF[:, 0, k, :], PF[:, 1, k, :], PF[:, 0, k - 1, :], PF[:, 1, k - 1, :], PF[:, 0, 1, :], PF[:, 1, 1, :],
                 T("t1")[:], T("t2")[:], [kS], [kS])
        for k in range(2, 8):
            cmul(PN[:, 0, k, :], PN[:, 1, k, :], PN[:, 0, k - 1, :], PN[:, 1, k - 1, :], PN[:, 0, 1, :], PN[:, 1, 1, :],
                 T("t1")[:], T("t2")[:], [kS], [kS])
        ts_(T("nr")[:], PF[:, 0, 1, :], -1.0, ALU.add)
        tt_(T("den")[:], lr, lr, ALU.mult)
        tt_(T("t1")[:], li, li, ALU.mult)
        tt_(T("den")[:], T("den")[:], T("t1")[:], ALU.add)
        V(lambda: nc.vector.reciprocal(T("rden")[:], T("den")[:]), [kS], [kS])
        tt_(T("t1")[:], T("nr")[:], lr, ALU.mult)
        tt_(T("t2")[:], PF[:, 1, 1, :], li, ALU.mult)
        tt_(T("t1")[:], T("t1")[:], T("t2")[:], ALU.add)
        tt_(T("fr")[:], T("t1")[:], T("rden")[:], ALU.mult)
        tt_(T("t1")[:], PF[:, 1, 1, :], lr, ALU.mult)
        tt_(T("t2")[:], T("nr")[:], li, ALU.mult)
        tt_(T("t1")[:], T("t1")[:], T("t2")[:], ALU.subtract)
        tt_(T("fi")[:], T("t1")[:], T("rden")[:], ALU.mult)
        Bb = sb(tt, "Bb", [128, 2, G, H])
        bt1 = sb(tt, "bt1", [128, G, H])
        bt2 = sb(tt, "bt2", [128, G, H])
        frb = T("fr")[:].unsqueeze(2).to_broadcast([128, G, H])
        fib = T("fi")[:].unsqueeze(2).to_broadcast([128, G, H])
        cmul(Bb[:, 0], Bb[:, 1], frb, fib, BA[:, 0], BA[:, 1], bt1[:], bt2[:], [kS, "BA" + sfx], [kS])

        r8 = sb(tt, "r8", [128, 32])
        e1 = sb(tt, "e1", [128, 2, 32])
        e2 = sb(tt, "e2", [128, 2, 32])
        rt1 = sb(tt, "rt1", [128, 32, 32])
        rt2 = sb(tt, "rt2", [128, 32, 32])
        kR = "rot" + sfx
        V(lambda: nc.vector.reciprocal(T("rm8")[:], T("mag8")[:]), [kS], [kS])
        tt_(T("u_re")[:], PF[:, 0, 8, :], T("rm8")[:], ALU.mult)
        tt_(T("u_im")[:], PF[:, 1, 8, :], T("rm8")[:], ALU.mult)
        for gpar in range(2):
            sl = slice(gpar * 64, (gpar + 1) * 64)

            def par(t):
                return t[sl, :].rearrange("q (a two) -> q a two", two=2)[:, :, gpar]
            V(lambda: nc.vector.tensor_copy(r8[sl, :], par(T("mag8"))), [kS], [kR])
            V(lambda: nc.vector.tensor_copy(e1[sl, 0, :], par(T("u_re"))), [kS], [kR])
            V(lambda: nc.vector.tensor_copy(e1[sl, 1, :], par(T("u_im"))), [kS], [kR])
        V(lambda: nc.vector.memset(O.COS[:, :, 0:1], 1.0), [kROT], [kROT])
        V(lambda: nc.vector.memset(O.SIN[:, :, 0:1], 0.0), [kROT], [kROT])
        cur, nxt = e1, e2
        m = 1
        while m <= 64:
            n = min(m, 65 - m)
            cb = lambda t, ri: t[:, ri, :].unsqueeze(2).to_broadcast([128, 32, n])
            cmul(O.COS[:, :, m:m + n], O.SIN[:, :, m:m + n], O.COS[:, :, 0:n], O.SIN[:, :, 0:n], cb(cur, 0), cb(cur, 1),
                 rt1[:, :, 0:n], rt2[:, :, 0:n], [kROT, kR], [kROT, kR])
            if m < 64:
                cmul(nxt[:, 0, :], nxt[:, 1, :], cur[:, 0, :], cur[:, 1, :], cur[:, 0, :], cur[:, 1, :],
                     rt1[:, 0, :], rt2[:, 0, :], [kR], [kR])
                cur, nxt = nxt, cur
            m *= 2
        V(lambda: nc.vector.memset(O.MULT[:, :, 0:1], 0.0), [kROT], [kROT])
        V(lambda: nc.vector.tensor_copy(O.MULT[:, :, 1:65], r8[:].unsqueeze(2).to_broadcast([128, 32, 64])), [kR, kROT], [kROT])

        NQ = 8
        big = [sb(tt, "big%d" % i, [128, NQ, 8, H]) for i in range(8)]
        Bsr, Bsi, Cjr, Cji, T1, T2, W1, W2 = big
        kB = ["big%d" % i + sfx for i in range(8)]
        tmpM = sb(tt, "tmpM", [128, 128])
        msk = sb(tt, "msk", [128, 128])
        S.dma("sp", msk[:], (I.maskp if dirn == 0 else I.maskq)[:, :], writes=["msk" + sfx])
        tabB, tabC = (PN, PF) if dirn == 0 else (PF, PN)
        SH = [128, NQ, 8, H]
        for qd in range(G // NQ):
            g0 = qd * NQ

            def powb(tab, ri, k0=0):
                return tab[:, ri, k0:k0 + 8, g0:g0 + NQ].rearrange("q s g -> q g s").unsqueeze(3).to_broadcast(SH)

            def gb_(tab3, ri):
                return tab3[:, ri, g0:g0 + NQ, :].unsqueeze(2).to_broadcast(SH)

            def gs_(t2d):
                return t2d[:, g0:g0 + NQ].unsqueeze(2).unsqueeze(3).to_broadcast(SH)
            rk = [kS, "CA" + sfx] + kB
            cmul(Bsr[:], Bsi[:], powb(tabB, 0), powb(tabB, 1), gb_(Bb, 0), gb_(Bb, 1), T1[:], T2[:], rk, kB[0:2] + kB[4:6])
            cmul(Cjr[:], Cji[:], powb(tabC, 0), powb(tabC, 1), gb_(CA, 0), gb_(CA, 1), T1[:], T2[:], rk, kB[2:4] + kB[4:6])
            V(lambda: nc.vector.tensor_single_scalar(W1[:], Bsi[:], -1.0, ALU.mult), [kB[1]], [kB[6]])
            for gl in range(NQ):
                g = g0 + gl
                bank = K.ps[3 + (gl // 4) % 2]
                bk = "ps%d" % (3 + (gl // 4) % 2)
                o = bank[:, (gl % 4) * 128:(gl % 4 + 1) * 128]
                f = lambda t: t[0:64, gl, :, :].rearrange("q s h -> q (s h)")
                V(lambda: nc.tensor.matmul(o, f(Bsr), f(Cjr), start=True, stop=False), [kB[0], kB[2]], [bk], "pe")
                V(lambda: nc.tensor.matmul(o, f(W1), f(Cji), start=False, stop=True), [kB[6], kB[3]], [bk], "pe")
                if dirn == 0:
                    V(lambda: nc.vector.tensor_tensor(tmpM[:], o, msk[:], ALU.mult), [bk, "msk" + sfx], ["tmpM" + sfx])
                    V(lambda: nc.vector.scalar_tensor_tensor(O.Mop[:, g, :], K.ident[:], O.tab[:, 4, g:g + 1], tmpM[:],
                                                             ALU.mult, ALU.add),
                      ["tmpM" + sfx, "ident"] + allt, [kM])
                else:
                    V(lambda: nc.vector.tensor_tensor(O.Mop[:, g, :], o, msk[:], ALU.mult), [bk, "msk" + sfx], [kM])
            if dirn == 0:
                cmul(W1[:], W2[:], gs_(PF[:, 0, 7, :]), gs_(PF[:, 1, 7, :]), Bsr[:], Bsi[:], T1[:], T2[:],
                     [kS] + kB, kB[4:8])
                wsa = (W1, W2)
                kW = (kB[6], kB[7])
            else:
                wsa = (Bsr, Bsi)
                kW = (kB[0], kB[1])
            for g4 in range(NQ // 4):
                bank, bk = K.ps[5], "ps5"
                for gq in range(4):
                    gl = g4 * 4 + gq
                    for ri in range(2):
                        V(lambda: nc.tensor.transpose(bank[:, (gq * 2 + ri) * 64:(gq * 2 + ri + 1) * 64],
                                                      wsa[ri][0:64, gl, :, :].rearrange("q s h -> q (s h)"),
                                                      K.ident[0:64, 0:64]),
                          [kW[ri], "ident"], [bk], "pe")
                gg = g0 + g4 * 4
                V(lambda: nc.scalar.copy(O.WS[:, gg:gg + 4, :, :].rearrange("q g r p -> q (g r p)"), bank[:, :]),
                  [bk], [kWS], "act")
            kk = 1 if dirn == 0 else 8
            cmul(W1[:], W2[:], gs_(PF[:, 0, kk, :]), gs_(PF[:, 1, kk, :]), Cjr[:], Cji[:], T1[:], T2[:],
                 [kS] + kB, kB[4:8])
            p0 = g0 // 2
            for gpar in range(2):
                sl = slice(gpar * 64, (gpar + 1) * 64)
                osl = slice((1 - gpar) * 64, (2 - gpar) * 64)
                src = lambda t: t[sl].rearrange("q (a two) s h -> q a two (s h)", two=2)[:, :, gpar, :]
                V(lambda: nc.vector.tensor_copy(O.YS[sl, p0:p0 + NQ // 2, 0, gpar * 128:(gpar + 1) * 128], src(W1)),
                  [kB[6]], [kYS])
                V(lambda: nc.vector.tensor_single_scalar(O.YS[sl, p0:p0 + NQ // 2, 1, gpar * 128:(gpar + 1) * 128], src(W2),
                                                         -1.0, ALU.mult), [kB[7]], [kYS])
                V(lambda: nc.gpsimd.memset(O.YS[sl, p0:p0 + NQ // 2, :, (1 - gpar) * 128:(2 - gpar) * 128], 0.0),
                  [], [kYS], "pool")
        if True:
            dbg_dump(K, "tab" + sfx, O.tab[:], allt)
            dbg_dump(K, "PF" + sfx, PF[:], [kS])
            dbg_dump(K, "PN" + sfx, PN[:], [kS])
            dbg_dump(K, "sincos" + sfx, T("sin")[:], [kS])
            dbg_dump(K, "Bb" + sfx, Bb[:], [kS])
            dbg_dump(K, "CA" + sfx, CA[:], ["CA" + sfx])
            dbg_dump(K, "COS" + sfx, O.COS[:], [kROT, kR])
            dbg_dump(K, "SIN" + sfx, O.SIN[:], [kROT, kR])
            dbg_dump(K, "MULT" + sfx, O.MULT[:], [kROT])
            dbg_dump(K, "Mop" + sfx, O.Mop[:], [kM])
            dbg_dump(K, "WS" + sfx, O.WS[:], [kWS])
            dbg_dump(K, "YS" + sfx, O.YS[:], [kYS])
        S.barrier()
    O.keys = (kWS, kM, kYS, kROT, allt)
    return O


def s5_pass(K, I, Sc, st, dirn, O):
    nc, S = K.nc, K.S
    sfx = "_%d" % dirn
    kWS, kM, kYS, kROT, allt = O.keys
    rev = (dirn == 1)

    def sb(name, shape, dt=F32):
        return st.enter_context(nc.sbuf_tensor(name + sfx, list(shape), dt))

    def V(fn, r, w, eng="dve"):
        S.op(eng, fn, reads=r, writes=w)

    xb = [sb("xb%d" % p, [64, 8, 128]) for p in range(2)]
    xr = [sb("xr%d" % p, [64, 8, 8, 16]) for p in range(2)]
    Rbs = [sb("Rb%d" % p, [128, G, 64], BF16) for p in range(2)]
    Z = sb("Z", [128, 2, 32, 65])
    ZS = sb("ZS", [128, 2, 32, 65])
    ft1 = sb("ft1", [128, 8, 64])
    ft2 = sb("ft2", [128, 8, 64])
    bt1 = sb("pbt1", [128, 32, 64])
    bt2 = sb("pbt2", [128, 32, 64])
    hist = sb("hist", [128, 2, 32, 64], BF16)
    ybuf = sb("ybuf", [64, 8, 128])
    y1b = sb("y1b", [64, 8, 128])
    ygb = sb("ygb", [64, 8, 128], BF16)
    V(lambda: nc.vector.memset(Z[:], 0.0), [], ["Z"])

    if dirn == 0:
        blocks = [(0, 32, True, False)] + [(NCTX + 512 * k, 64, False, k >= 3) for k in range(8)]
    else:
        blocks = [(OFF_CTX1, 32, True, False)] + [(NCTX + 512 * k, 64, False, True) for k in (7, 6, 5, 4, 3)]
    qcnt = [0]

    def front(bi):
        t0, nch, is_ctx, want = blocks[bi]
        isc, ish = (2, 3) if is_ctx else (0, 1)
        Rb, kRb = Rbs[bi % 2], "Rb%d" % (bi % 2)
        for hq in range(8):
            qp = qcnt[0] % 2
            qcnt[0] += 1
            xb_, xr_, kxb, kxr = xb[qp], xr[qp], "xb%d" % qp, "xr%d" % qp
            S.dma("sp", xb_[0:nch, :, :], I.seq[t0:t0 + nch * 8, hq * 128:(hq + 1) * 128].rearrange("(c s) d -> c s d", s=8),
                  writes=[kxb])
            V(lambda: nc.gpsimd.tensor_copy(xr_[0:nch].rearrange("c g s h -> c s g h"),
                                            xb_[0:nch].rearrange("c s (g h) -> c s g h", h=16)), [kxb], [kxr], "pool")
            for gq in range(1):
                bank, bk = (K.ps[0], "ps0") if hq % 2 == 0 else (K.psf, "psb")
                for gl in range(8):
                    g_loc = gl
                    V(lambda: nc.tensor.transpose(bank[:, gl * 64:gl * 64 + nch],
                                                  xr_[0:nch, g_loc, :, :].rearrange("c s h -> c (s h)"),
                                                  K.ident[0:nch, 0:nch]), [kxr, "ident"], [bk], "pe")
                for gl in range(8):
                    g = hq * 8 + gl
                    V(lambda: nc.scalar.activation(Rb[:, g, 0:nch], bank[:, gl * 64:gl * 64 + nch], AF.Identity,
                                                   bias=O.tab[:, ish, g:g + 1], scale=O.tab[:, isc, g:g + 1]),
                      [bk] + allt, [kRb], "act")

    def mid(bi):
        t0, nch, is_ctx, want = blocks[bi]
        Rb, kRb = Rbs[bi % 2], "Rb%d" % (bi % 2)
        sl0 = 65 - nch
        if is_ctx:
            V(lambda: nc.vector.memset(Z[:], 0.0), [], ["Z"])
        for sbk in range(4):
            pre, pim = K.ps[2 + 2 * (sbk % 2)], K.ps[3 + 2 * (sbk % 2)]
            kre, kim = "ps%d" % (2 + 2 * (sbk % 2)), "ps%d" % (3 + 2 * (sbk % 2))
            for pl in range(8):
                pair = sbk * 8 + pl
                for gpar in range(2):
                    g = 2 * pair + gpar
                    for ri, pst, kk in ((0, pre, kre), (1, pim, kim)):
                        V(lambda: nc.tensor.matmul(pst[gpar * 64:(gpar + 1) * 64, pl * 64:pl * 64 + nch], O.WS[:, g, ri, :],
                                                   Rb[:, g, 0:nch], start=True, stop=True), [kWS, kRb], [kk], "pe")

            def pv(pst):
                v = pst[:, :].rearrange("q (p c) -> q p c", c=64)[:, :, 0:nch]
                return rev_last(v) if rev else v
            ps_ = slice(sbk * 8, (sbk + 1) * 8)
            cs = O.COS[:, ps_, sl0:65]
            sn = O.SIN[:, ps_, sl0:65]
            a, b = ft1[:, :, 0:nch], ft2[:, :, 0:nch]
            V(lambda: nc.vector.tensor_tensor(a, pv(pre), cs, ALU.mult), [kre, kROT], ["ft1"])
            V(lambda: nc.vector.tensor_tensor(b, pv(pim), sn, ALU.mult), [kim, kROT], ["ft2"])
            V(lambda: nc.vector.tensor_tensor(Z[:, 0, ps_, sl0:65], a, b, ALU.add), ["ft1", "ft2"], ["Z"])
            V(lambda: nc.vector.tensor_tensor(a, pv(pim), cs, ALU.mult), [kim, kROT], ["ft1"])
            V(lambda: nc.vector.tensor_tensor(b, pv(pre), sn, ALU.mult), [kre, kROT], ["ft2"])
            V(lambda: nc.vector.tensor_tensor(Z[:, 1, ps_, sl0:65], a, b, ALU.subtract), ["ft1", "ft2"], ["Z"])
        for ri in range(2):
            V(lambda: nc.vector.tensor_tensor_scan(ZS[:, ri].rearrange("q p c -> q (p c)"),
                                                   O.MULT[:].rearrange("q p c -> q (p c)"),
                                                   Z[:, ri].rearrange("q p c -> q (p c)"), 0.0, ALU.mult, ALU.add),
              ["Z", kROT], ["ZS"])
        c64, s64 = O.COS[:, :, 64:65], O.SIN[:, :, 64:65]
        zr, zi = ZS[:, 0, :, 64:65], ZS[:, 1, :, 64:65]
        a, b = bt1[:, :, 0:1], bt2[:, :, 0:1]
        if want:
            rng_ = slice(sl0 - 1, 64)

            def sv(t):
                return rev_last(t) if rev else t
            hr, hi = sv(ZS[:, 0, :, rng_]), sv(ZS[:, 1, :, rng_])
            hc, hs = sv(O.COS[:, :, rng_]), sv(O.SIN[:, :, rng_])
            ha, hb = bt1[:, :, 0:nch], bt2[:, :, 0:nch]
            V(lambda: nc.vector.tensor_tensor(ha, hr, hc, ALU.mult), ["ZS", kROT], ["bt1"])
            V(lambda: nc.vector.tensor_tensor(hb, hi, hs, ALU.mult), ["ZS", kROT], ["bt2"])
            V(lambda: nc.vector.tensor_tensor(hist[:, 0, :, 0:nch], ha, hb, ALU.subtract), ["bt1", "bt2"], ["hist"])
            V(lambda: nc.vector.tensor_tensor(ha, hi, hc, ALU.mult), ["ZS", kROT], ["bt1"])
            V(lambda: nc.vector.tensor_tensor(hb, hr, hs, ALU.mult), ["ZS", kROT], ["bt2"])
            V(lambda: nc.vector.tensor_tensor(hist[:, 1, :, 0:nch], ha, hb, ALU.add), ["bt1", "bt2"], ["hist"])
        V(lambda: nc.vector.tensor_tensor(a, zr, c64, ALU.mult), ["ZS", kROT], ["bt1"])
        V(lambda: nc.vector.tensor_tensor(b, zi, s64, ALU.mult), ["ZS", kROT], ["bt2"])
        V(lambda: nc.vector.tensor_tensor(Z[:, 0, :, 0:1], a, b, ALU.subtract), ["bt1", "bt2"], ["Z"])
        V(lambda: nc.vector.tensor_tensor(a, zi, c64, ALU.mult), ["ZS", kROT], ["bt1"])
        V(lambda: nc.vector.tensor_tensor(b, zr, s64, ALU.mult), ["ZS", kROT], ["bt2"])
        V(lambda: nc.vector.tensor_tensor(Z[:, 1, :, 0:1], a, b, ALU.add), ["bt1", "bt2"], ["Z"])

    def back(bi):
        t0, nch, is_ctx, want = blocks[bi]
        if not want:
            return
        Rb, kRb = Rbs[bi % 2], "Rb%d" % (bi % 2)
        dom0 = t0 - OFF_HALO
        for gb in range(8):
            ydst = Sc.Y1[dom0:dom0 + 512, gb * 128:(gb + 1) * 128].rearrange("(c j) d -> c j d", j=8)
            kY1 = "Y1_%d_%d" % (dom0, gb)
            if dirn == 1:
                S.dma("sp", y1b[:], ydst, reads=[kY1], writes=["y1b"])
            for h4 in range(2):
                bank, bk = (K.ps[1], "ps1") if h4 == 0 else (K.ps[6], "ps6")
                for pq in range(2):
                    pair = gb * 4 + h4 * 2 + pq
                    o = bank[0:64, pq * 256:(pq + 1) * 256]
                    V(lambda: nc.tensor.matmul(o, hist[:, 0, pair, :], O.YS[:, pair, 0, :], start=True, stop=False),
                      ["hist", kYS], [bk], "pe")
                    V(lambda: nc.tensor.matmul(o, hist[:, 1, pair, :], O.YS[:, pair, 1, :], start=False, stop=False),
                      ["hist", kYS], [bk], "pe")
                    for gpar in range(2):
                        g = 2 * pair + gpar
                        V(lambda: nc.tensor.matmul(bank[0:64, pq * 256 + gpar * 128:pq * 256 + (gpar + 1) * 128],
                                                   Rb[:, g, :], O.Mop[:, g, :], start=False, stop=(gpar == 1)),
                          [kRb, kM], [bk], "pe")
                pin = bank[0:64, :].rearrange("c (g j h) -> c g j h", g=4, j=8)

                def dv(t):
                    return t[:, :, h4 * 64:(h4 + 1) * 64].rearrange("c j (g h) -> c g j h", g=4)
                if dirn == 0:
                    V(lambda: nc.scalar.copy(dv(ybuf), pin), [bk], ["ybuf"], "act")
                else:
                    V(lambda: nc.vector.tensor_tensor(dv(ybuf), pin, dv(y1b), ALU.add), [bk, "y1b"], ["ybuf"])
                    V(lambda: nc.scalar.activation(dv(ygb), dv(ybuf), AF.Gelu_apprx_tanh), ["ybuf"], ["ygb"], "act")
            if dirn == 0:
                S.dma("pool", ydst, ybuf[:], reads=["ybuf"], writes=[kY1])
            else:
                S.dma("pool", Sc.YG[dom0:dom0 + 512, gb * 128:(gb + 1) * 128].rearrange("(c j) d -> c j d", j=8), ygb[:],
                      reads=["ygb"], writes=["YG_%d" % dom0])

    front(0)
    for bi in range(len(blocks)):
        mid(bi)
        if bi + 1 < len(blocks):
            front(bi + 1)
        back(bi)


def tail_consts(K, I, Sc, st, l):
    nc, S = K.nc, K.S
    C = Ctx()
    sfx = "_t%d" % l
    C.sfx = sfx
    C.lng = bload(K, st, "lng" + sfx, I.ln_g[l, 0, :])
    C.lnb = bload(K, st, "lnb" + sfx, I.ln_b[l, 0, :])
    C.A2 = bload(K, st, "A2" + sfx, Sc.MOD[l, 0, 4 * D:5 * D], reads=["MOD"])
    C.B2 = bload(K, st, "B2" + sfx, Sc.MOD[l, 0, 3 * D:4 * D], reads=["MOD"])
    kA, kB = "A2" + sfx, "B2" + sfx
    S.op("dve", lambda: nc.vector.tensor_scalar_add(C.A2[:], C.A2[:], 1.0), reads=[kA], writes=[kA])
    S.op("dve", lambda: nc.vector.tensor_tensor(C.B2[:], C.lnb[:], C.A2[:], ALU.mult), reads=[kA, kB, "lnb" + sfx], writes=["B2tmp" + sfx, kB] if False else [kB])
    sh2 = bload(K, st, "sh2" + sfx, Sc.MOD[l, 0, 3 * D:4 * D], reads=["MOD"])
    S.op("dve", lambda: nc.vector.tensor_tensor(C.B2[:], C.B2[:], sh2[:], ALU.add), reads=[kB, "sh2" + sfx], writes=[kB])
    S.op("dve", lambda: nc.vector.tensor_tensor(C.A2[:], C.A2[:], C.lng[:], ALU.mult), reads=[kA, "lng" + sfx], writes=[kA])
    C.rw = st.enter_context(nc.sbuf_tensor("rw" + sfx, [128, 8, E], F32))
    S.dma("sp", C.rw[:], I.router_w.rearrange("(k p) e -> p k e", p=128), writes=["rw" + sfx])
    C.rb = bload(K, st, "rb" + sfx, I.router_b[:])
    C.keys = ["lng" + sfx, "lnb" + sfx, kA, kB, "rw" + sfx, "rb" + sfx]

    def sb(name, shape, dt=F32):
        return st.enter_context(nc.sbuf_tensor(name + sfx, list(shape), dt))
    C.par = []
    for pp in range(2):
        Wk = Ctx()
        Wk.sfx = sfx + "p%d" % pp
        sbp = lambda name, shape, dt=F32: sb(name + "p%d" % pp, shape, dt)
        Wk.st6 = sbp("st6", [128, 12])
        Wk.mv = sbp("mv", [128, 2])
        Wk.rstd = sbp("rstd", [128, 1])
        Wk.xn = sbp("xn", [128, D])
        Wk.x1 = sbp("x1t", [128, D])
        Wk.h2 = sbp("h2t", [128, D])
        Wk.h2Tb = sbp("h2Tb", [128, 8, 128], BF16)
        Wk.h2Tf = sbp("h2Tf", [128, 8, 128])
        Wk.r = {n: sbp("r_" + n, [128, 16]) for n in ("lg", "e", "mk1", "t", "e2", "mk2", "w", "cmb")}
        Wk.q = {n: sbp("q_" + n, [128, 4]) for n in ("m1", "m2", "gs", "gm", "rg")}
        Wk.s1 = {n: sbp("s_" + n, [128, 1]) for n in ("mx", "nmx", "gmax")}
        for a in ("lng", "lnb", "A2", "B2", "rw", "rb", "keys"):
            setattr(Wk, a, getattr(C, a))
        C.par.append(Wk)
    return C


def layer_norm_tile(K, C, z, zkey, lnsfx):
    nc, S = K.nc, K.S
    k = lambda n: n + lnsfx
    S.op("dve", lambda: nc.vector.bn_stats(C.st6[:, 0:6], z[:, 0:512]), reads=[zkey], writes=[k("st6")])
    S.op("dve", lambda: nc.vector.bn_stats(C.st6[:, 6:12], z[:, 512:1024]), reads=[zkey], writes=[k("st6")])
    S.op("dve", lambda: nc.vector.bn_aggr(C.mv[:], C.st6[:]), reads=[k("st6")], writes=[k("mv")])
    S.op("dve", lambda: nc.vector.tensor_scalar_add(C.rstd[:], C.mv[:, 1:2], EPS), reads=[k("mv")], writes=[k("rstd")])
    S.op("act", lambda: nc.scalar.sqrt(C.rstd[:], C.rstd[:]), reads=[k("rstd")], writes=[k("rstd")])
    S.op("dve", lambda: nc.vector.reciprocal(C.rstd[:], C.rstd[:]), reads=[k("rstd")], writes=[k("rstd")])
    S.op("dve", lambda: nc.vector.tensor_scalar(C.xn[:], z[:], C.mv[:, 0:1], C.rstd[:, 0:1], ALU.subtract, ALU.mult),
         reads=[zkey, k("mv"), k("rstd")], writes=[getattr(C, "xnkey", k("xn"))])


def tail_tile(K, I, Sc, C, l, z, zkey, t0):
    nc, S = K.nc, K.S
    C = C.par[(t0 // 128) % 2]
    pp = (t0 // 128) % 2
    sfx = C.sfx
    k = lambda n: n + sfx
    layer_norm_tile(K, C, z, zkey, sfx)
    S.op("pool", lambda: nc.gpsimd.tensor_tensor(C.x1[:], C.xn[:], C.lng[:], ALU.mult), reads=[k("xn")] + C.keys, writes=[k("x1")])
    S.op("pool", lambda: nc.gpsimd.tensor_tensor(C.x1[:], C.x1[:], C.lnb[:], ALU.add), reads=[k("x1")] + C.keys, writes=[k("x1")])
    S.dma("pool", Sc.X1[l][t0:t0 + 128, :], C.x1[:], reads=[k("x1")], writes=["X1_%d_%d" % (l, t0)])
    if GLUSUB < 4:
        return
    S.op("dve", lambda: nc.vector.tensor_tensor(C.h2[:], C.xn[:], C.A2[:], ALU.mult), reads=[k("xn")] + C.keys, writes=[k("h2")])
    S.op("dve", lambda: nc.vector.tensor_tensor(C.h2[:], C.h2[:], C.B2[:], ALU.add), reads=[k("h2")] + C.keys, writes=[k("h2")])
    for hb in range(2):
        bank, bk = K.ps[2 + hb + 3 * pp], "ps%d" % (2 + hb + 3 * pp)
        for q in range(4):
            kc = hb * 4 + q
            S.op("pe", lambda: nc.tensor.transpose(bank[:, q * 128:(q + 1) * 128], C.h2[:, kc * 128:(kc + 1) * 128], K.ident[:]),
                 reads=[k("h2"), "ident"], writes=[bk])
        S.op("dve", lambda: nc.vector.tensor_copy(C.h2Tf[:, hb * 4:(hb + 1) * 4, :].rearrange("p k t -> p (k t)"), bank[:, :]),
             reads=[bk], writes=[k("h2Tf")])
        S.op("act", lambda: nc.scalar.copy(C.h2Tb[:, hb * 4:(hb + 1) * 4, :].rearrange("p k t -> p (k t)"),
                                           C.h2Tf[:, hb * 4:(hb + 1) * 4, :].rearrange("p k t -> p (k t)")),
             reads=[k("h2Tf")], writes=[k("h2Tb")])
    for kc in range(8 if os.environ.get("NOH2T") is None else 0):
        S.dma("sp", Sc.H2T[l][kc, :, t0:t0 + 128], C.h2Tb[:, kc, :], reads=[k("h2Tb")], writes=["H2T_%d_%d" % (l, t0)])
    return


def tail_tile_B(K, I, Sc, C, l, t0):
    nc, S = K.nc, K.S
    C = C.par[(t0 // 128) % 2]
    sfx = C.sfx
    k = lambda n: n + sfx
    lgp, lk = K.ps[4], "ps4"
    for kc in range(8):
        S.op("pe", lambda: nc.tensor.matmul(lgp[:, 0:E], C.h2Tf[:, kc, :], C.rw[:, kc, :], start=(kc == 0), stop=(kc == 7)),
             reads=[k("h2Tf")] + C.keys, writes=[lk])
    r, q, s1 = C.r, C.q, C.s1
    kr = k("route")

    def V(fn, rd=(), eng="dve"):
        S.op(eng, fn, reads=[kr] + list(rd), writes=[kr])
    e3 = lambda t: t[:].rearrange("p (g i) -> p g i", i=4)
    b3 = lambda t: t[:].unsqueeze(2).to_broadcast([128, 4, 4])
    AXX = mybir.AxisListType.X
    V(lambda: nc.vector.tensor_tensor(r["lg"][:], lgp[:, 0:E], C.rb[:], ALU.add), [lk] + C.keys)
    V(lambda: nc.vector.tensor_reduce(s1["mx"][:], r["lg"][:], AXX, ALU.max))
    V(lambda: nc.vector.tensor_scalar_mul(s1["nmx"][:], s1["mx"][:], -1.0))
    V(lambda: nc.scalar.activation(r["e"][:], r["lg"][:], AF.Exp, bias=s1["nmx"][:, 0:1]), eng="act")
    V(lambda: nc.vector.tensor_reduce(q["m1"][:], e3(r["e"]), AXX, ALU.max))
    V(lambda: nc.vector.tensor_tensor(e3(r["mk1"]), e3(r["e"]), b3(q["m1"]), ALU.is_equal))
    V(lambda: nc.vector.tensor_tensor(r["t"][:], r["mk1"][:], r["e"][:], ALU.mult))
    V(lambda: nc.vector.tensor_tensor(r["e2"][:], r["e"][:], r["t"][:], ALU.subtract))
    V(lambda: nc.vector.tensor_reduce(q["m2"][:], e3(r["e2"]), AXX, ALU.max))
    V(lambda: nc.vector.tensor_tensor(e3(r["mk2"]), e3(r["e2"]), b3(q["m2"]), ALU.is_equal))
    V(lambda: nc.vector.tensor_tensor(q["gs"][:], q["m1"][:], q["m2"][:], ALU.add))
    V(lambda: nc.vector.tensor_reduce(s1["gmax"][:], q["gs"][:], AXX, ALU.max))
    V(lambda: nc.vector.tensor_tensor(q["gm"][:], q["gs"][:], s1["gmax"][:].to_broadcast([128, 4]), ALU.is_equal))
    V(lambda: nc.vector.reciprocal(q["rg"][:], q["gs"][:]))
    V(lambda: nc.vector.tensor_tensor(q["rg"][:], q["rg"][:], q["gm"][:], ALU.mult))
    V(lambda: nc.vector.tensor_tensor(r["w"][:], r["mk1"][:], r["mk2"][:], ALU.add))
    V(lambda: nc.vector.tensor_tensor(r["w"][:], r["w"][:], r["e"][:], ALU.mult))
    V(lambda: nc.vector.tensor_tensor(e3(r["cmb"]), e3(r["w"]), b3(q["rg"]), ALU.mult))
    S.dma("pool", Sc.CMB[l][t0:t0 + 128, :], r["cmb"][:], reads=[kr], writes=["CMB_%d_%d" % (l, t0)])


def phase_glu(K, I, Sc):
    nc, S = K.nc, K.S
    with ExitStack() as st:
        def sb(name, shape, dt=F32):
            return st.enter_context(nc.sbuf_tensor(name, list(shape), dt))
        C = tail_consts(K, I, Sc, st, 0)
        g1B = bload(K, st, "g1B", Sc.MOD[0, 0, 2 * D:3 * D], reads=["MOD"])
        wv = sb("wv", [128, 8, D], BF16)
        wg = sb("wgl", [128, 8, D], BF16)
        stage = [sb("gstage%d" % i, [128, D]) for i in range(2)]
        it = 0
        for wsrc, wdst, kn in ((I.w_val, wv, "wv"), (I.w_gate, wg, "wgl")):
            for kc in range(8):
                sg, sk = stage[it % 2], "gstage%d" % (it % 2)
                it += 1
                S.dma("sp", sg[:], wsrc[kc * 128:(kc + 1) * 128, :], writes=[sk])
                S.op("act", lambda: nc.scalar.copy(wdst[:, kc, :], sg[:]), reads=[sk], writes=[kn])
        dbuf = [[sb("ygt%d" % p, [128, D], BF16), sb("yT%d" % p, [128, 8, 128], BF16), sb("xt_g%d" % p, [128, D]),
                 sb("sgm%d" % p, [128, 512]), sb("mt%d" % p, [128, D]), sb("zt%d" % p, [128, D])] for p in range(3)]
        def glu_front(tt):
            t0 = tt * 128
            blk = (t0 // 512) * 512
            ygt, yT, xt, sgm, mt, zt = dbuf[tt % 3]
            kq = lambda n: n + "%d" % (tt % 3)
            S.dma("sp", ygt[:], Sc.YG[t0:t0 + 128, :], reads=["YG_%d" % blk], writes=[kq("ygt")])
            S.dma("sp", xt[:], I.seq[OFF_HALO + t0:OFF_HALO + t0 + 128, :], writes=[kq("xt_g")])
            for kc in range(8):
                S.op("pe", lambda: nc.tensor.transpose(K.psb[:, kc * 128:(kc + 1) * 128], ygt[:, kc * 128:(kc + 1) * 128], K.identb[:]),
                     reads=[kq("ygt"), "identb"], writes=["psb"])
            S.op("act", lambda: nc.scalar.copy(yT[:].rearrange("p k t -> p (k t)"), K.psb[:, :]), reads=["psb"], writes=[kq("yT")])
            for nh in range(2):
                for kc in range(8):
                    S.op("pe", lambda: nc.tensor.matmul(K.ps[0][:, :], yT[:, kc, :], wv[:, kc, nh * 512:(nh + 1) * 512],
                                                        start=(kc == 0), stop=(kc == 7)), reads=[kq("yT"), "wv"], writes=["ps0"])
                for kc in range(8):
                    S.op("pe", lambda: nc.tensor.matmul(K.ps[1][:, :], yT[:, kc, :], wg[:, kc, nh * 512:(nh + 1) * 512],
                                                        start=(kc == 0), stop=(kc == 7)), reads=[kq("yT"), "wgl"], writes=["ps1"])
                S.op("act", lambda: nc.scalar.activation(sgm[:], K.ps[1][:, :], AF.Sigmoid), reads=["ps1"], writes=[kq("sgm")])
                S.op("dve", lambda: nc.vector.tensor_tensor(mt[:, nh * 512:(nh + 1) * 512], K.ps[0][:, :], sgm[:], ALU.mult),
                     reads=["ps0", kq("sgm")], writes=[kq("mt")])
            S.op("dve", lambda: nc.vector.tensor_tensor(mt[:], mt[:], g1B[:], ALU.mult), reads=[kq("mt"), "g1B"], writes=[kq("mt")])
            S.op("dve", lambda: nc.vector.scalar_tensor_tensor(zt[:], xt[:], ALPHA, mt[:], ALU.mult, ALU.add),
                 reads=[kq("xt_g"), kq("mt")], writes=[kq("zt")])
            return zt, kq("zt"), t0
        NTL = NT0 // 128
        fq = [glu_front(0)]
        prev = None
        for tt in range(NTL):
            if tt + 1 < NTL:
                fq.append(glu_front(tt + 1))
            cur = fq.pop(0)
            tail_tile(K, I, Sc, C, 0, cur[0], cur[1], cur[2])
            if prev is not None:
                tail_tile_B(K, I, Sc, C, 0, prev)
            prev = cur[2]
        tail_tile_B(K, I, Sc, C, 0, prev)
        S.barrier()


def phase_moe(K, I, Sc, l, T, dst, dst_off):
    nc, S = K.nc, K.S
    TH = T // 2
    NTT = TH // 128
    chunks = []
    o = 0
    while o < TH:
        n = min(512, TH - o)
        chunks.append((o, n))
        o += n
    sfx = "_m%d" % l
    with ExitStack() as st:
        def sb(name, shape, dt=F32):
            return st.enter_context(nc.sbuf_tensor(name + sfx, list(shape), dt))
        g2B = bload(K, st, "g2B" + sfx, Sc.MOD[l, 0, 5 * D:6 * D], reads=["MOD"])
        lng = bload(K, st, "lng2" + sfx, I.ln_g[l, 1, :])
        lnb = bload(K, st, "lnb2" + sfx, I.ln_b[l, 1, :])
        ckeys = ["g2B" + sfx, "lng2" + sfx, "lnb2" + sfx]
        h2T = sb("h2T", [128, 8, TH], BF16)
        aT = sb("aT", [128, 8, TH], BF16)
        acc = sb("acc", [128, NTT, D])
        cmb = sb("cmb", [128, NTT, E])
        W = [[sb("w%d_%d" % (p, j), [128, 8, D], BF16) for j in range(3)] for p in range(2)]
        NSTG = 3
        stage = [sb("stage%d" % i, [128, D]) for i in range(NSTG)]
        sgt = [sb("sgt%d" % i, [128, 512]) for i in range(2)]
        L = Ctx()
        L.st6 = sb("st6", [128, 12]); L.mv = sb("mv", [128, 2]); L.rstd = sb("rstd", [128, 1])
        L.xn = stage[2]
        L.xnkey = "stage2" + sfx
        x1t, zt = stage[0], stage[1]
        kx1, kzt = "stage0" + sfx, "stage1" + sfx
        wsrc = (I.moe_wg, I.moe_wu, I.moe_wd)
        sidx = [0]

        def emit_piece(e, par, j, kc):
            i = sidx[0] % NSTG
            sidx[0] += 1
            sk = "stage%d" % i + sfx
            S.dma("sp", stage[i][:], wsrc[j][l, e, kc * 128:(kc + 1) * 128, :], writes=[sk])
            wk = "w%d_%d_%d" % (par, j, kc) + sfx
            sel = (j * 8 + kc) % 4
            if sel in (0, 2):
                S.op("act", lambda: nc.scalar.copy(W[par][j][:, kc, :], stage[i][:]), reads=[sk], writes=[wk])
            elif sel == 1:
                S.op("dve", lambda: nc.vector.tensor_copy(W[par][j][:, kc, :], stage[i][:]), reads=[sk], writes=[wk])
            else:
                S.op("pool", lambda: nc.gpsimd.tensor_copy(W[par][j][:, kc, :], stage[i][:]), reads=[sk], writes=[wk])

        def load_expert(e, par):
            for j in range(3):
                for kc in range(8):
                    emit_piece(e, par, j, kc)

        for half in range(2):
            hoff = half * TH
            hk = ["H2T_%d_%d" % (l, hoff + t * 128) for t in range(NTT)]
            S.dma("sp", h2T[:], Sc.H2T[l][:, :, hoff:hoff + TH].rearrange("k p t -> p k t"), reads=hk, writes=["h2T" + sfx])
            ck = ["CMB_%d_%d" % (l, hoff + t * 128) for t in range(NTT)]
            S.dma("sp", cmb[:], Sc.CMB[l][hoff:hoff + TH, :].rearrange("(t p) e -> p t e", p=128), reads=ck, writes=["cmb" + sfx])
            if half == 0:
                load_expert(0, 0)
            for e in range(E):
                par = e % 2
                if e + 1 < E:
                    pend = [(e + 1, 1 - par, j, kc) for j in range(3) for kc in range(8)]
                elif half == 0:
                    pend = [(0, 1 - par, j, kc) for j in range(3) for kc in range(8)]
                else:
                    pend = []
                wg_, wu_, wd_ = W[par]
                wkeys = lambda j: ["w%d_%d_%d" % (par, j, kc) + sfx for kc in range(8)]
                ci = 0
                for fc in range(8):
                    for (co, cn) in chunks:
                        pg, pu = K.ps[ci % 2], K.ps[2 + ci % 2]
                        kg, ku = "ps%d" % (ci % 2), "ps%d" % (2 + ci % 2)
                        sg_, sgk = sgt[ci % 2], "sgt%d" % (ci % 2) + sfx
                        ci += 1
                        for kc in range(8):
                            S.op("pe", lambda: nc.tensor.matmul(pg[:, 0:cn], wg_[:, kc, fc * 128:(fc + 1) * 128], h2T[:, kc, co:co + cn],
                                                                start=(kc == 0), stop=(kc == 7)),
                                 reads=["w%d_0_%d" % (par, kc) + sfx, "h2T" + sfx], writes=[kg])
                        for kc in range(8):
                            S.op("pe", lambda: nc.tensor.matmul(pu[:, 0:cn], wu_[:, kc, fc * 128:(fc + 1) * 128], h2T[:, kc, co:co + cn],
                                                                start=(kc == 0), stop=(kc == 7)),
                                 reads=["w%d_1_%d" % (par, kc) + sfx, "h2T" + sfx], writes=[ku])
                        S.op("act", lambda: nc.scalar.activation(sg_[:, 0:cn], pg[:, 0:cn], AF.Silu), reads=[kg], writes=[sgk])
                        S.op("dve", lambda: nc.vector.tensor_tensor(aT[:, fc, co:co + cn], sg_[:, 0:cn], pu[:, 0:cn], ALU.mult),
                             reads=[sgk, ku], writes=["aT%d" % fc + sfx])
                        for _ in range(2 if len(chunks) == 2 else 1):
                            if pend:
                                emit_piece(*pend.pop(0))
                while pend:
                    emit_piece(*pend.pop(0))
                di = 0
                for tt in range(NTT):
                    for dh in range(2):
                        pd, kd = K.ps[4 + di % 3], "ps%d" % (4 + di % 3)
                        di += 1
                        for fc in range(8):
                            S.op("pe", lambda: nc.tensor.matmul(pd[:, :], aT[:, fc, tt * 128:(tt + 1) * 128], wd_[:, fc, dh * 512:(dh + 1) * 512],
                                                                start=(fc == 0), stop=(fc == 7)),
                                 reads=["aT%d" % fc + sfx, "w%d_2_%d" % (par, fc) + sfx], writes=[kd])
                        ak = "acc%d" % tt + sfx
                        av = acc[:, tt, dh * 512:(dh + 1) * 512]
                        if e == 0:
                            S.op("dve", lambda: nc.vector.tensor_scalar(av, pd[:, :], cmb[:, tt, e:e + 1], None, ALU.mult),
                                 reads=[kd, "cmb" + sfx], writes=[ak])
                        else:
                            S.op("dve", lambda: nc.vector.scalar_tensor_tensor(av, pd[:, :], cmb[:, tt, e:e + 1], av, ALU.mult, ALU.add),
                                 reads=[kd, "cmb" + sfx, ak], writes=[ak])
            for tt in range(NTT):
                t0 = hoff + tt * 128
                ak = "acc%d" % tt + sfx
                S.dma("sp", x1t[:], Sc.X1[l][t0:t0 + 128, :], reads=["X1_%d_%d" % (l, t0)], writes=[kx1])
                S.op("dve", lambda: nc.vector.tensor_tensor(acc[:, tt, :], acc[:, tt, :], g2B[:], ALU.mult), reads=[ak] + ckeys, writes=[ak])
                S.op("dve", lambda: nc.vector.scalar_tensor_tensor(zt[:], x1t[:], ALPHA, acc[:, tt, :], ALU.mult, ALU.add),
                     reads=[kx1, ak], writes=[kzt])
                layer_norm_tile(K, L, zt, kzt, sfx)
                S.op("dve", lambda: nc.vector.tensor_tensor(zt[:], L.xn[:], lng[:], ALU.mult), reads=[L.xnkey] + ckeys, writes=[kzt])
                S.op("dve", lambda: nc.vector.tensor_tensor(zt[:], zt[:], lnb[:], ALU.add), reads=[kzt] + ckeys, writes=[kzt])
                S.dma("pool", dst[dst_off + t0:dst_off + t0 + 128, :], zt[:], reads=[kzt], writes=["X2_%d_%d" % (l, t0)])
        S.barrier()


def phase_pool(K, I, Sc):
    nc, S = K.nc, K.S
    NR, NCOL = 40, 64
    PW = 84
    PR = 49
    with ExitStack() as st:
        def sb(name, shape, dt=F32):
            return st.enter_context(nc.sbuf_tensor(name + "_p", list(shape), dt))

        def V(fn, r, w, eng="dve"):
            S.op(eng, fn, reads=r, writes=w)
        C = tail_consts(K, I, Sc, st, 1)
        sc1 = sb("sc1col", [128, 8])
        S.dma("sp", sc1[:], Sc.MOD[1, 0, D:2 * D].rearrange("(k p) -> p k", p=128), reads=["MOD"], writes=["sc1col"],
              allow_slow_non_contiguous=True)
        V(lambda: nc.vector.tensor_scalar_add(sc1[:], sc1[:], 1.0), ["sc1col"], ["sc1col"])
        GS = bload(K, st, "GS_p", Sc.MOD[1, 0, 2 * D:3 * D], reads=["MOD"])
        psc = bload(K, st, "psc_p", I.pool_scale[:])
        V(lambda: nc.vector.tensor_tensor(GS[:], GS[:], psc[:], ALU.mult), ["GS_p", "psc_p"], ["GS_p"])
        INVC = bload(K, st, "INVC_p", I.invc.rearrange("a b -> (a b)"))
        INVR = bload(K, st, "INVR_p", I.invr.rearrange("a b -> (a b)"))
        SEL = bload(K, st, "SEL_p", I.sel[:])
        pwf = sb("pwf", [128, 4, 2, 256])
        pw = sb("pw", [128, 4, 2, 256], BF16)
        S.dma("sp", pwf[:], I.pool_w.rearrange("g (c p) d -> p g c d", p=128), writes=["pwf"])
        V(lambda: nc.scalar.copy(pw[:].rearrange("p g c d -> p (g c d)"), pwf[:].rearrange("p g c d -> p (g c d)")),
          ["pwf"], ["pw"], "act")
        pooledT = sb("pooledT", [128, 8, NOWN], BF16)
        inner = ExitStack()
        sbi = lambda name, shape, dt=F32: inner.enter_context(nc.sbuf_tensor(name + "_p", list(shape), dt))
        xcol = sbi("xcol", [128, NT0 // 128, 128])
        xp = sbi("xp", [128, PR, PW])
        A = sbi("pA", [128, PR, PW])
        B = sbi("pB", [128, PR, PW])
        Bc = sbi("pBc", [128, PR, NCOL])
        mean = sbi("pmean", [128, 32, NCOL])
        V(lambda: nc.gpsimd.memset(xp[:], 0.0), [], ["xp"], "pool")
        V(lambda: nc.gpsimd.memset(Bc[:], 0.0), [], ["pBc"], "pool")
        allx2 = ["X2_0_%d" % (t * 128) for t in range(NT0 // 128)]
        for dc in range(8):
            gk = dc // 2
            kk = POOLK[gk]
            steps = [s_ for s_ in (1, 2, 4, 8) if s_ < kk]
            S.dma("sp", xcol[:], Sc.X2[:, dc * 128:(dc + 1) * 128].rearrange("(t p) d -> p t d", p=128), reads=allx2, writes=["xcol"])
            for q in range(5):
                bank, bk = K.ps[q % 2], "ps%d" % (q % 2)
                for j in range(4):
                    V(lambda: nc.tensor.transpose(bank[:, j * 128:(j + 1) * 128], xcol[:, q * 4 + j, :], K.ident[:]),
                      ["xcol", "ident"], [bk], "pe")
                V(lambda: nc.scalar.copy(xp[:, q * 8:(q + 1) * 8, 8:8 + NCOL], bank[:, :].rearrange("p (r c) -> p r c", c=NCOL)),
                  [bk], ["xp"], "act")
            cur, ck = xp, "xp"
            W = PW
            bufs = [(A, "pA"), (B, "pB")]
            for si, s_ in enumerate(steps):
                nb, nk = bufs[si % 2]
                V(lambda: nc.vector.tensor_tensor(nb[:, 0:NR, 0:W - s_], cur[:, 0:NR, 0:W - s_], cur[:, 0:NR, s_:W], ALU.add),
                  [ck], [nk])
                cur, ck = nb, nk
                W -= s_
            o0 = 8 - kk // 2
            V(lambda: nc.vector.tensor_scalar(Bc[:, 0:NR, :], cur[:, 0:NR, o0:o0 + NCOL], SEL[:, 0:1], None, ALU.mult),
              [ck, "SEL_p"], ["pBc"])
            V(lambda: nc.vector.scalar_tensor_tensor(Bc[:, 0:NR, :], cur[:, 0:NR, o0 + 1:o0 + 1 + NCOL], SEL[:, 1:2], Bc[:, 0:NR, :],
                                                     ALU.mult, ALU.add), [ck, "SEL_p", "pBc"], ["pBc"])
            V(lambda: nc.vector.tensor_tensor(Bc[:, 0:NR, :], Bc[:, 0:NR, :],
                                              INVC[:, gk * 64:(gk + 1) * 64].unsqueeze(1).to_broadcast([128, NR, NCOL]), ALU.mult),
              ["pBc", "INVC_p"], ["pBc"])
            cur, ck = Bc, "pBc"
            Hh = PR
            bufs = [(A, "pA"), (B, "pB")]
            for si, s_ in enumerate(steps):
                nb, nk = bufs[si % 2]
                V(lambda: nc.vector.tensor_tensor(nb[:, 0:Hh - s_, 0:NCOL], cur[:, 0:Hh - s_, 0:NCOL], cur[:, s_:Hh, 0:NCOL], ALU.add),
                  [ck], [nk])
                cur, ck = nb, nk
                Hh -= s_
            r0 = 8 - kk // 2
            V(lambda: nc.vector.tensor_scalar(mean[:], cur[:, r0:r0 + 32, 0:NCOL], SEL[:, 0:1], None, ALU.mult), [ck, "SEL_p"], ["pmean"])
            V(lambda: nc.vector.scalar_tensor_tensor(mean[:], cur[:, r0 + 1:r0 + 33, 0:NCOL], SEL[:, 1:2], mean[:], ALU.mult, ALU.add),
              [ck, "SEL_p", "pmean"], ["pmean"])
            V(lambda: nc.vector.tensor_tensor(mean[:], mean[:],
                                              INVR[:, gk * 32:(gk + 1) * 32].unsqueeze(2).to_broadcast([128, 32, NCOL]), ALU.mult),
              ["pmean", "INVR_p"], ["pmean"])
            V(lambda: nc.vector.tensor_tensor(mean[:], mean[:], xp[:, 8:40, 8:8 + NCOL], ALU.subtract), ["pmean", "xp"], ["pmean"])
            V(lambda: nc.scalar.activation(pooledT[:, dc, :].rearrange("p (r c) -> p r c", c=NCOL), mean[:], AF.Identity,
                                           scale=sc1[:, dc:dc + 1]), ["pmean", "sc1col"], ["pooledT%d" % dc], "act")
        S.barrier()
        inner.close()
        pbuf = [[sb("x2t%d" % p, [128, D]), sb("mtp%d" % p, [128, D]), sb("ztp%d" % p, [128, D])] for p in range(3)]
        def pool_front(tt):
            t0 = tt * 128
            x2t, mt, zt = pbuf[tt % 3]
            kq = lambda n: n + "%d" % (tt % 3)
            S.dma("sp", x2t[:], Sc.X2[NHALO + t0:NHALO + t0 + 128, :], reads=["X2_0_%d" % (NHALO + t0)], writes=[kq("x2t_p")])
            for gk in range(4):
                bank, bk = K.ps[gk // 2], "ps%d" % (gk // 2)
                for cc in range(2):
                    V(lambda: nc.tensor.matmul(bank[:, (gk % 2) * 256:(gk % 2 + 1) * 256], pooledT[:, 2 * gk + cc, t0:t0 + 128],
                                               pw[:, gk, cc, :], start=(cc == 0), stop=(cc == 1)),
                      ["pooledT%d" % (2 * gk + cc), "pw"], [bk], "pe")
            for hb in range(2):
                V(lambda: nc.vector.tensor_tensor(mt[:, hb * 512:(hb + 1) * 512], K.ps[hb][:, :], GS[:, hb * 512:(hb + 1) * 512], ALU.mult),
                  ["ps%d" % hb, "GS_p"], [kq("mtp")])
            V(lambda: nc.vector.scalar_tensor_tensor(zt[:], x2t[:], ALPHA, mt[:], ALU.mult, ALU.add), [kq("x2t_p"), kq("mtp")], [kq("ztp")])
            return zt, kq("ztp"), t0
        NTL = NOWN // 128
        fq = [pool_front(0)]
        prev = None
        for tt in range(NTL):
            if tt + 1 < NTL:
                fq.append(pool_front(tt + 1))
            cur = fq.pop(0)
            tail_tile(K, I, Sc, C, 1, cur[0], cur[1], cur[2])
            if prev is not None:
                tail_tile_B(K, I, Sc, C, 1, prev)
            prev = cur[2]
        tail_tile_B(K, I, Sc, C, 1, prev)
        S.barrier()
```
